# Optimizing a Trainium2 kernel written in Bass

```python
import math
import jax, jax.numpy as jnp
from jax import lax
import numpy as np

D_MODEL = 1024
BATCH = 4
SEQ = 4096
DEPTH = 4

MEM_LEN = 256
HEAD_DIM = 64
EPS = 1e-6
CONV_CH = D_MODEL // 4
MOBA_WIDTH = D_MODEL // 2
N_MOBA_HEADS = MOBA_WIDTH // HEAD_DIM
GLA_WIDTH = D_MODEL // 4
N_GLA_HEADS = 4
GLA_DV = GLA_WIDTH // N_GLA_HEADS
GLA_DK = GLA_DV // 2
GLA_KEY_WIDTH = N_GLA_HEADS * GLA_DK
GLA_GATE_RANK = 16
GLA_GATE_NORMALIZER = 16.0
GLA_CHUNK = 64
MOBA_BLOCK = 256
MOBA_TOPK = 3
MOBA_QCHUNK = 32
CONV_K = 3
N_XATTN_HEADS = 4
XATTN_WIDTH = N_XATTN_HEADS * HEAD_DIM
D_FF = 2752
IN_SPLITS = (CONV_CH, CONV_CH, CONV_CH,
             MOBA_WIDTH, MOBA_WIDTH, MOBA_WIDTH,
             GLA_KEY_WIDTH, GLA_KEY_WIDTH, GLA_WIDTH, GLA_WIDTH, GLA_GATE_RANK)
IN_PROJ_WIDTH = sum(IN_SPLITS)
MIX_WIDTH = CONV_CH + MOBA_WIDTH + GLA_WIDTH

kernel_name = "hybrid_conv_moba_gla_trunk"


def rmsnorm(x, g):
    xf = x.astype(jnp.float32)
    xf = xf * lax.rsqrt(jnp.mean(xf * xf, axis=-1, keepdims=True) + EPS)
    return xf.astype(x.dtype) * g


def causal_dwconv(u, w, b):
    T = u.shape[1]
    up = jnp.pad(u, ((0, 0), (CONV_K - 1, 0), (0, 0)))
    return sum(up[:, j:j + T] * w[j] for j in range(CONV_K)) + b


def alibi_slopes(n_heads):
    return jnp.asarray(2.0 ** (-8.0 * np.arange(1, n_heads + 1) / n_heads), dtype=jnp.float32)


def moba_attention(q, k, v, slopes):
    Bsz, T, H, hd = q.shape
    nblk = -(-T // MOBA_BLOCK)
    Tp = nblk * MOBA_BLOCK
    pad = ((0, 0), (0, Tp - T), (0, 0), (0, 0))
    q = jnp.pad(q, pad).transpose(0, 2, 1, 3) * (hd ** -0.5)
    k = jnp.pad(k, pad).transpose(0, 2, 1, 3)
    v = jnp.pad(v, pad).transpose(0, 2, 1, 3)
    kb = k.reshape(Bsz, H, nblk, MOBA_BLOCK, hd)
    vb = v.reshape(Bsz, H, nblk, MOBA_BLOCK, hd)
    kmean = jnp.mean(kb.astype(jnp.float32), axis=3)
    gate = jnp.einsum('bhtd,bhnd->bhtn', q.astype(jnp.float32), kmean)
    qblk = jnp.arange(Tp) // MOBA_BLOCK
    past = jnp.arange(nblk)[None, :] < qblk[:, None]
    gate = jnp.where(past, gate, -jnp.inf)
    topk = min(MOBA_TOPK, nblk)
    _, gidx = lax.top_k(gate, topk)
    bi = jnp.arange(Bsz)[:, None, None, None]
    hi = jnp.arange(H)[None, :, None, None]
    sl = slopes[None, :, None, None]

    def chunk(c):
        s0 = c * MOBA_QCHUNK
        qc = lax.dynamic_slice_in_dim(q, s0, MOBA_QCHUNK, axis=2)
        idx = lax.dynamic_slice_in_dim(gidx, s0, MOBA_QCHUNK, axis=2)
        qpos = s0 + jnp.arange(MOBA_QCHUNK)
        valid = jnp.arange(topk)[None, :] < (qpos // MOBA_BLOCK)[:, None]
        ksel = kb[bi, hi, idx]
        vsel = vb[bi, hi, idx]
        kpos_sel = idx[..., None] * MOBA_BLOCK + jnp.arange(MOBA_BLOCK)
        s_sel = jnp.einsum('bhqd,bhqnkd->bhqnk', qc, ksel).astype(jnp.float32)
        s_sel = s_sel - sl[..., None] * (qpos[:, None, None] - kpos_sel).astype(jnp.float32)
        s_sel = jnp.where(valid[None, None, :, :, None], s_sel, -jnp.inf)
        own = s0 // MOBA_BLOCK
        kown = lax.dynamic_slice_in_dim(k, own * MOBA_BLOCK, MOBA_BLOCK, axis=2)
        vown = lax.dynamic_slice_in_dim(v, own * MOBA_BLOCK, MOBA_BLOCK, axis=2)
        kpos_own = own * MOBA_BLOCK + jnp.arange(MOBA_BLOCK)
        rel = (qpos[:, None] - kpos_own[None, :])
        s_own = jnp.einsum('bhqd,bhkd->bhqk', qc, kown).astype(jnp.float32)
        s_own = s_own - sl * rel.astype(jnp.float32)
        s_own = jnp.where(rel >= 0, s_own, -jnp.inf)
        scores = jnp.concatenate(
            [s_sel.reshape(Bsz, H, MOBA_QCHUNK, topk * MOBA_BLOCK), s_own], axis=-1)
        p = jax.nn.softmax(scores, axis=-1).astype(v.dtype)
        p_sel = p[..., :topk * MOBA_BLOCK].reshape(Bsz, H, MOBA_QCHUNK, topk, MOBA_BLOCK)
        p_own = p[..., topk * MOBA_BLOCK:]
        return (jnp.einsum('bhqnk,bhqnkd->bhqd', p_sel, vsel)
                + jnp.einsum('bhqk,bhkd->bhqd', p_own, vown))

    nqc = Tp // MOBA_QCHUNK
    out = lax.map(chunk, jnp.arange(nqc))
    out = out.transpose(1, 0, 3, 2, 4).reshape(Bsz, Tp, H * hd)
    return out[:, :T]


def gla_attention(q, k, v, log_a):
    Bsz, T, H, dk = q.shape
    dv = v.shape[-1]
    n = T // GLA_CHUNK

    def to_chunks(a):
        return a.reshape(Bsz, n, GLA_CHUNK, H, a.shape[-1]).transpose(0, 3, 1, 2, 4)

    q = to_chunks(q) * (dk ** -0.5)
    k = to_chunks(k)
    vc = to_chunks(v)
    b = jnp.cumsum(to_chunks(log_a).astype(jnp.float32), axis=3)
    b_last = b[:, :, :, -1:, :]
    qd = q * jnp.exp(b)
    kd = k * jnp.exp(-b)
    kend = k * jnp.exp(b_last - b)
    causal = jnp.tril(jnp.ones((GLA_CHUNK, GLA_CHUNK), dtype=bool))
    A = jnp.where(causal, jnp.einsum('bhncd,bhnsd->bhncs', qd, kd), 0.0)
    o_intra = jnp.einsum('bhncs,bhnse->bhnce', A, vc)
    U = jnp.einsum('bhnsd,bhnse->bhnde', kend, vc)
    decay = jnp.exp(b_last[:, :, :, 0, :])

    def step(S, xs):
        g, u = xs
        return g[..., None] * S + u, S

    S0 = jnp.zeros((Bsz, H, dk, dv), dtype=U.dtype)
    _, S_prev = lax.scan(step, S0, (decay.transpose(2, 0, 1, 3), U.transpose(2, 0, 1, 3, 4)))
    S_prev = S_prev.transpose(1, 2, 0, 3, 4)
    o_inter = jnp.einsum('bhncd,bhnde->bhnce', qd, S_prev)
    o = (o_intra + o_inter).transpose(0, 2, 3, 1, 4).reshape(Bsz, T, H, dv)
    return o.astype(v.dtype)


def hybrid_mixer(h, w_in, sc_conv_w, sc_conv_b, gla_w_gate, gla_b_gate, gla_norm_g, w_out, slopes):
    Bsz, T, _ = h.shape
    proj = h @ w_in
    points = tuple(int(p) for p in np.cumsum(IN_SPLITS)[:-1])
    (c_b, c_c, c_h, m_q, m_k, m_v, g_q, g_k, g_v, g_o, g_lr) = jnp.split(proj, points, axis=-1)
    y_conv = c_b * causal_dwconv(c_c * c_h, sc_conv_w, sc_conv_b)
    hs = lambda a, nh: a.reshape(Bsz, T, nh, -1)
    y_moba = moba_attention(hs(m_q, N_MOBA_HEADS), hs(m_k, N_MOBA_HEADS), hs(m_v, N_MOBA_HEADS), slopes)
    log_a = jax.nn.log_sigmoid((g_lr @ gla_w_gate + gla_b_gate).astype(jnp.float32)) / GLA_GATE_NORMALIZER
    o = gla_attention(hs(g_q, N_GLA_HEADS), hs(g_k, N_GLA_HEADS), hs(g_v, N_GLA_HEADS),
                      hs(log_a, N_GLA_HEADS))
    y_gla = rmsnorm(o, gla_norm_g).reshape(Bsz, T, GLA_WIDTH) * jax.nn.silu(g_o)
    return jnp.concatenate([y_conv, y_moba, y_gla], axis=-1) @ w_out


def memory_cross_attention(h, mem_n, wq, wkv, wo):
    Bsz, T, _ = h.shape
    q = (h @ wq).reshape(Bsz, T, N_XATTN_HEADS, HEAD_DIM)
    k, v = jnp.split(mem_n @ wkv, 2, axis=-1)
    k = k.reshape(Bsz, -1, N_XATTN_HEADS, HEAD_DIM)
    v = v.reshape(Bsz, -1, N_XATTN_HEADS, HEAD_DIM)
    s = jnp.einsum('bthd,bmhd->bhtm', q, k).astype(jnp.float32) * (HEAD_DIM ** -0.5)
    p = jax.nn.softmax(s, axis=-1).astype(v.dtype)
    o = jnp.einsum('bhtm,bmhd->bthd', p, v).reshape(Bsz, T, XATTN_WIDTH)
    return o @ wo


def conv_ffn(h, w_up, conv_w, conv_b, w_down):
    u = causal_dwconv(h @ w_up, conv_w, conv_b)
    a, g = jnp.split(u, 2, axis=-1)
    return (jax.nn.silu(a) * g) @ w_down


def setup_inputs(seed: int = 0) -> dict:
    key = jax.random.key(seed)
    ks = jax.random.split(key, 24)
    f32 = jnp.float32

    def w(k, shape, fan_in):
        return jax.random.normal(k, shape, f32) * (fan_in ** -0.5)

    def gain(k, shape):
        return 1.0 + 0.05 * jax.random.normal(k, shape, f32)

    def bias(k, shape):
        return 0.02 * jax.random.normal(k, shape, f32)

    L = DEPTH
    return {
        "x": jax.random.normal(ks[0], (BATCH, SEQ, D_MODEL), f32),
        "mem": jax.random.normal(ks[1], (BATCH, MEM_LEN, D_MODEL), f32),
        "norm_mix_g": gain(ks[2], (L, D_MODEL)),
        "w_in": w(ks[3], (L, D_MODEL, IN_PROJ_WIDTH), D_MODEL),
        "sc_conv_w": w(ks[4], (L, CONV_K, CONV_CH), CONV_K),
        "sc_conv_b": bias(ks[5], (L, CONV_CH)),
        "gla_w_gate": w(ks[6], (L, GLA_GATE_RANK, GLA_KEY_WIDTH), GLA_GATE_RANK),
        "gla_b_gate": bias(ks[7], (L, GLA_KEY_WIDTH)),
        "gla_norm_g": gain(ks[8], (L, GLA_DV)),
        "w_out": w(ks[9], (L, MIX_WIDTH, D_MODEL), MIX_WIDTH),
        "norm_xattn_g": gain(ks[10], (L, D_MODEL)),
        "norm_mem_g": gain(ks[11], (L, D_MODEL)),
        "xattn_wq": w(ks[12], (L, D_MODEL, XATTN_WIDTH), D_MODEL),
        "xattn_wkv": w(ks[13], (L, D_MODEL, 2 * XATTN_WIDTH), D_MODEL),
        "xattn_wo": w(ks[14], (L, XATTN_WIDTH, D_MODEL), XATTN_WIDTH),
        "norm_ffn_g": gain(ks[15], (L, D_MODEL)),
        "ffn_w_up": w(ks[16], (L, D_MODEL, 2 * D_FF), D_MODEL),
        "ffn_conv_w": w(ks[17], (L, CONV_K, 2 * D_FF), CONV_K),
        "ffn_conv_b": bias(ks[18], (L, 2 * D_FF)),
        "ffn_w_down": w(ks[19], (L, D_FF, D_MODEL), D_FF),
        "final_norm_g": gain(ks[20], (D_MODEL,)),
    }


def reference(x, mem, norm_mix_g, w_in, sc_conv_w, sc_conv_b, gla_w_gate, gla_b_gate, gla_norm_g,
              w_out, norm_xattn_g, norm_mem_g, xattn_wq, xattn_wkv, xattn_wo, norm_ffn_g,
              ffn_w_up, ffn_conv_w, ffn_conv_b, ffn_w_down, final_norm_g):
    slopes = alibi_slopes(N_MOBA_HEADS)
    for l in range(DEPTH):
        h = rmsnorm(x, norm_mix_g[l])
        x = x + hybrid_mixer(h, w_in[l], sc_conv_w[l], sc_conv_b[l], gla_w_gate[l], gla_b_gate[l],
                             gla_norm_g[l], w_out[l], slopes)
        h = rmsnorm(x, norm_xattn_g[l])
        x = x + memory_cross_attention(h, rmsnorm(mem, norm_mem_g[l]), xattn_wq[l], xattn_wkv[l], xattn_wo[l])
        h = rmsnorm(x, norm_ffn_g[l])
        x = x + conv_ffn(h, ffn_w_up[l], ffn_conv_w[l], ffn_conv_b[l], ffn_w_down[l])
    return rmsnorm(x, final_norm_g)
```

```python
import numpy as np
from contextlib import ExitStack
import concourse.bass as bass
import concourse.mybir as mybir
from concourse.bass_utils import run_bass_kernel_spmd

F32 = mybir.dt.float32
BF16 = mybir.dt.bfloat16
AF = mybir.ActivationFunctionType
ALU = mybir.AluOpType
AX = mybir.AxisListType

ENGS = ["pe", "act", "dve", "pool", "sp"]
SAME_ENGINE_SYNC = True
NDMA_SEM = 24
DMAQ = ["sp", "pool"]

L_ALL = 4
T = 4096
D = 1024
DFF = 2752
NEG = -30000.0
EPS = 1e-6


class Buf:
    __slots__ = ("name", "w", "r")

    def __init__(self, name=""):
        self.name = name
        self.w = None
        self.r = {}


def bufs(n, name=""):
    return [Buf(name + str(i)) for i in range(n)]


class Prog:
    def __init__(self, nc, sems, dma_sems):
        self.nc = nc
        self.sems = sems
        self.dma_sems = dma_sems
        self.ops = {e: [] for e in ENGS}
        self.seen = {e: {} for e in ENGS}
        self.seen_d = {e: set() for e in ENGS}
        self.needed = {e: set() for e in ENGS}
        self.cnt = {e: 0 for e in ENGS}
        self.base = {e: 0 for e in ENGS}
        self.rankmap = {e: {} for e in ENGS}
        self.ndma = 0
        self.dma_slot_last = {}
        self.dma_q_count = {e: 0 for e in ENGS}
        self.dma_info = {}
        self.dma_slot_uses = {}
        self.outstanding = []
        self.nblock = 0
        self.ninstr = 0

    def _deps(self, reads, writes):
        deps = []
        for b in reads:
            if b.w is not None:
                deps.append(b.w)
        for b in writes:
            if b.w is not None:
                deps.append(b.w)
            deps.extend(b.r.values())
        return deps

    def _waits(self, eng, deps):
        waits = []
        seen = self.seen[eng]
        sd = self.seen_d[eng]
        for t in deps:
            if t[0] == "c":
                _, e2, idx = t
                if e2 == eng and (eng == "pe" or not SAME_ENGINE_SYNC):
                    continue
                if seen.get(e2, 0) >= idx:
                    continue
                seen[e2] = idx
                waits.append(t)
                self.needed[e2].add(idx)
            else:
                if t in sd:
                    continue
                sd.add(t)
                waits.append(t)
        return waits

    def _commit(self, tok, reads, writes):
        for b in writes:
            b.w = tok
            b.r = {}
        key = tok[1] if tok[0] == "c" else tok
        for b in reads:
            b.r[key] = tok

    def op(self, eng, fn, reads=(), writes=()):
        deps = self._deps(reads, writes)
        waits = self._waits(eng, deps)
        self.cnt[eng] += 1
        idx = self.cnt[eng]
        tok = ("c", eng, idx)
        self.ops[eng].append(["c", fn, waits, idx])
        self._commit(tok, reads, writes)
        return tok

    def dma(self, queue, fn, reads=(), writes=()):
        deps = self._deps(reads, writes)
        slot = self.dma_q_count[queue] % NDMA_SEM
        self.dma_q_count[queue] += 1
        prev = self.dma_slot_last.get((queue, slot))
        if prev is not None:
            deps.append(prev)
        waits = self._waits(queue, deps)
        self.ndma += 1
        tok = ("d", self.ndma)
        uses = self.dma_slot_uses.get((queue, slot), 0) + 1
        self.dma_slot_uses[(queue, slot)] = uses
        self.dma_info[tok] = (queue, slot, 16 * uses)
        self.dma_slot_last[(queue, slot)] = tok
        self.ops[queue].append(["d", fn, waits, tok])
        self._commit(tok, reads, writes)
        self.outstanding.append(tok)
        return tok

    def flush(self):
        w = self._waits("sp", list(self.outstanding))
        if w:
            self.ops["sp"].append(["w", None, w, None])
        rank = {}
        for e in ENGS:
            r = {}
            n = self.base[e]
            for i in sorted(self.needed[e]):
                n += 1
                r[i] = n
            rank[e] = r
        sems, dma_sems = self.sems, self.dma_sems

        def emit_engine(e, h):
            for kind, fn, waits, ident in self.ops[e]:
                for t in waits:
                    if t[0] == "c":
                        h.wait_ge(sems[t[1]], rank[t[1]][t[2]])
                    else:
                        q, slot, val = self.dma_info[t]
                        h.wait_ge(dma_sems[q][slot], val)
                if kind == "w":
                    continue
                ins = fn(h)
                self.ninstr += 1
                if kind == "c":
                    if ident in self.needed[e]:
                        ins.then_inc(sems[e], 1)
                else:
                    q, slot, val = self.dma_info[ident]
                    ins.then_inc(dma_sems[q][slot], 16)

        with self.nc.Block() as block:
            @block.sync
            def _(e):
                emit_engine("sp", e)

            @block.tensor
            def _(e):
                emit_engine("pe", e)

            @block.scalar
            def _(e):
                emit_engine("act", e)

            @block.vector
            def _(e):
                emit_engine("dve", e)

            @block.gpsimd
            def _(e):
                emit_engine("pool", e)
        for e in ENGS:
            self.base[e] += len(self.needed[e])
            self.needed[e] = set()
            self.ops[e] = []
        for e in ENGS:
            for e2 in ENGS:
                self.seen[e][e2] = self.cnt[e2]
            self.seen_d[e] = set()
        self.dma_slot_last = {}
        self.outstanding = []
        self._all_done = True
        self.nblock += 1


_orig_deps = Prog._deps


def _deps_epoch(self, reads, writes):
    deps = _orig_deps(self, reads, writes)
    out = []
    for t in deps:
        if t[0] == "d":
            if t[1] <= getattr(self, "_dma_done_upto", 0):
                continue
        out.append(t)
    return out


Prog._deps = _deps_epoch
_orig_flush = Prog.flush


def _flush2(self):
    _orig_flush(self)
    self._dma_done_upto = self.ndma


Prog.flush = _flush2


def host_consts():
    c = {}
    k = np.arange(128)[:, None, None]
    ktl = np.arange(4)[None, :, None]
    q = np.arange(512)[None, None, :]
    c["cm"] = np.where(q >= ktl * 128 + k, 0.0, NEG).astype(np.float32).reshape(128, 2048)
    qt = np.arange(32)[None, :, None]
    j = np.arange(16)[None, None, :]
    qblk = qt // 2
    ones = np.ones((128, 1, 1))
    c["pm"] = (np.where(j < qblk, 0.0, NEG) * ones).astype(np.float32).reshape(128, 512)
    c["ownfix"] = (np.where(j >= qblk, 0.0, -1e9) * ones).astype(np.float32).reshape(128, 512)
    c["alibase"] = (-256.0 * np.maximum(qblk - j, 0) * ones).astype(np.float32).reshape(128, 512)
    kk = np.arange(T)
    kst = np.zeros((8, 32, T), np.float32)
    for h in range(8):
        slope = 2.0 ** (-(h + 1))
        for jb in range(16):
            kst[h, jb, jb * 256:(jb + 1) * 256] = 1.0
        kst[h, 16, :] = -slope
        kst[h, 17, :] = slope * (kk % 256)
    c["kstat"] = kst
    qs = np.zeros((16, T), np.float32)
    qs[0] = kk % 256
    qs[1] = 1.0
    c["qstat"] = qs
    s = np.arange(128)[:, None]
    t = np.arange(128)[None, :]
    same = (s // 64) == (t // 64)
    c["ltri"] = np.where(same & (s <= t), -1.0 / 16, 0.0).astype(np.float32)
    c["lrev"] = np.where(same & (s > t), -1.0 / 16, 0.0).astype(np.float32)
    tri = np.where(same & (s <= t), 1.0, 0.0).astype(np.float32)
    c["tri01"] = np.tile(tri[:, None, :], (1, 4, 1)).reshape(128, 512)
    c["hm"] = (np.arange(128)[:, None] // 32 == np.arange(4)[None, :]).astype(np.float32)
    c["ident"] = np.eye(128, dtype=np.float32)
    return c


CONST_SHAPES = {"cm": [128, 2048], "pm": [128, 512], "ownfix": [128, 512], "alibase": [128, 512],
                "kstat": [8, 32, T], "qstat": [16, T], "ltri": [128, 128], "lrev": [128, 128],
                "tri01": [128, 512], "hm": [128, 4], "ident": [128, 128]}

PARAM_SHAPES = {
    "xT": [D, T], "memT": [D, 256],
    "w_in": [L_ALL, D, 3088], "w_out": [L_ALL, D, D], "xattn_wq": [L_ALL, D, 256],
    "xattn_wkv": [L_ALL, D, 512], "xattn_wo": [L_ALL, 256, D], "ffn_w_up": [L_ALL, D, 2 * DFF],
    "ffn_w_down": [L_ALL, DFF, D],
    "gains": [128, (L_ALL * 4 + 1) * 8], "scw": [128, L_ALL * 2 * 4], "fcw": [128, L_ALL * 44 * 4],
    "wg_aug": [32, L_ALL * 128], "gn": [64, L_ALL],
}


def host_params(inp):
    p = {}
    L = L_ALL
    g = np.zeros((128, L * 4 + 1, 8), np.float32)
    for l in range(L):
        for k, nm in enumerate(["norm_mix_g", "norm_xattn_g", "norm_mem_g", "norm_ffn_g"]):
            g[:, l * 4 + k, :] = inp[nm][l].reshape(8, 128).T
    g[:, L * 4, :] = inp["final_norm_g"].reshape(8, 128).T
    p["gains"] = g.reshape(128, -1)
    sc = np.zeros((128, L, 2, 4), np.float32)
    for l in range(L):
        for ch in range(2):
            sc[:, l, ch, 0:3] = inp["sc_conv_w"][l][:, ch * 128:(ch + 1) * 128].T
            sc[:, l, ch, 3] = inp["sc_conv_b"][l][ch * 128:(ch + 1) * 128]
    p["scw"] = sc.reshape(128, -1)
    fc = np.zeros((128, L, 44, 4), np.float32)
    for l in range(L):
        for half in range(2):
            for jc in range(22):
                lo = jc * 128
                n = min(128, DFF - lo)
                cols = slice(half * DFF + lo, half * DFF + lo + n)
                fc[:n, l, half * 22 + jc, 0:3] = inp["ffn_conv_w"][l][:, cols].T
                fc[:n, l, half * 22 + jc, 3] = inp["ffn_conv_b"][l][cols]
    p["fcw"] = fc.reshape(128, -1)
    wg = np.zeros((32, L, 128), np.float32)
    for l in range(L):
        wg[0:16, l, :] = inp["gla_w_gate"][l]
        wg[16, l, :] = inp["gla_b_gate"][l]
    p["wg_aug"] = wg.reshape(32, -1)
    p["gn"] = np.ascontiguousarray(inp["gla_norm_g"].T)
    return p


def build(n_layers=L_ALL, debug=False, max_phase=9):
    nc = bass.Bass("TRN2", target_bir_lowering=False)
    I = {}
    for nm, shp in list(PARAM_SHAPES.items()) + list(CONST_SHAPES.items()):
        I[nm] = nc.dram_tensor(nm, shp, F32, kind="ExternalInput").ap()
    outT = nc.dram_tensor("outT", [D, T], F32, kind="ExternalOutput").ap()

    def scratch(nm, shp, dt):
        return nc.dram_tensor(nm, shp, dt, kind="Internal").ap()
    xs = scratch("xs", [8, 128, T], F32)
    q_d = scratch("q_d", [8, 64, T], BF16)
    k_d = scratch("k_d", [8, 64, T], BF16)
    v_d = scratch("v_d", [32, 128, 512], BF16)
    yconv_d = scratch("yconv_d", [2, 128, T], BF16)
    ymoba_d = scratch("ymoba_d", [8, 64, T], BF16)
    ygla_d = scratch("ygla_d", [4, 64, T], BF16)
    qd_d = scratch("qd_d", [128, T], BF16)
    kd_d = scratch("kd_d", [128, T], BF16)
    kend_d = scratch("kend_d", [32, 128, 512], BF16)
    vg_d = scratch("vg_d", [32, 128, 256], BF16)
    sg_d = scratch("sg_d", [4, 64, T], BF16)
    dbg = {}
    if debug:
        dbg["yconv"] = nc.dram_tensor("dbg_yconv", [2, 128, T], BF16, kind="ExternalOutput").ap()
        dbg["ymoba"] = nc.dram_tensor("dbg_ymoba", [8, 64, T], BF16, kind="ExternalOutput").ap()
        dbg["ygla"] = nc.dram_tensor("dbg_ygla", [4, 64, T], BF16, kind="ExternalOutput").ap()
        dbg["x2"] = nc.dram_tensor("dbg_x2", [8, 128, T], F32, kind="ExternalOutput").ap()

    B_xs = bufs(8, "xs")
    B_q = [bufs(8, "q%d_" % h) for h in range(8)]
    B_k = [bufs(8, "k%d_" % h) for h in range(8)]
    B_v = bufs(32, "v")
    B_yconv = bufs(8, "yc")
    B_ymoba = [bufs(8, "ym%d_" % h) for h in range(8)]
    B_ygla = bufs(32, "yg")
    B_qd = bufs(8, "qd")
    B_kd = bufs(8, "kd")
    B_kend = bufs(32, "kend")
    B_vg = bufs(32, "vg")
    B_sg = bufs(8, "sg")

    with ExitStack() as top:
        sems = {e: top.enter_context(nc.semaphore("s_" + e)) for e in ENGS}
        dma_sems = {q: [top.enter_context(nc.semaphore("d_%s_%d" % (q, i))) for i in range(NDMA_SEM)] for q in DMAQ}
        P = Prog(nc, sems, dma_sems)

        uid = [0]

        def sbt(es, name, shape, dt):
            uid[0] += 1
            return es.enter_context(nc.sbuf_tensor("sb%d_%s" % (uid[0], name), shape, dt))

        gains = sbt(top, "gains", [128, (L_ALL * 4 + 1) * 8], F32)
        scw = sbt(top, "scw", [128, L_ALL * 2 * 4], F32)
        fcw = sbt(top, "fcw", [128, L_ALL * 44 * 4], F32)
        wg = sbt(top, "wg", [32, L_ALL * 128], F32)
        gn = sbt(top, "gn", [64, L_ALL], F32)
        hm = sbt(top, "hm", [128, 4], F32)
        ident = sbt(top, "ident", [128, 128], BF16)
        ones_bf = sbt(top, "ones_bf", [128, 128], BF16)
        ones_f = sbt(top, "ones_f", [128, 64], F32)
        dec = sbt(top, "dec", [128, 64], F32)
        B_dec = Buf("dec")
        B_const = Buf("const")
        banks = [top.enter_context(nc.psum_tensor("bank%d" % i, [128, 512], F32)) for i in range(8)]
        B_bank = bufs(8, "bank")

        class Rot:
            def __init__(self, ids):
                self.ids = ids
                self.i = 0

            def next(self):
                k = self.ids[self.i % len(self.ids)]
                self.i += 1
                return banks[k], B_bank[k]

        def MM(out, lhsT, rhs, start, stop, reads, writes, **kw):
            P.op("pe", lambda e: e.matmul(out, lhsT=lhsT, rhs=rhs, start=start, stop=stop, **kw), reads, writes)

        def ACT(out, in_, func, reads, writes, **kw):
            P.op("act", lambda e: e.activation(out=out, in_=in_, func=func, **kw), reads, writes)

        def TT(out, in0, in1, op, reads, writes, eng="dve"):
            P.op(eng, lambda e: e.tensor_tensor(out=out, in0=in0, in1=in1, op=op), reads, writes)

        def STT(out, in0, scalar, in1, op0, op1, reads, writes):
            P.op("dve", lambda e: e.scalar_tensor_tensor(out=out, in0=in0, scalar=scalar, in1=in1, op0=op0, op1=op1), reads, writes)

        def TS(out, in0, s1, s2, op0, op1, reads, writes, eng="dve"):
            if op1 is None:
                P.op(eng, lambda e: e.tensor_scalar(out=out, in0=in0, scalar1=s1, scalar2=None, op0=op0), reads, writes)
            else:
                P.op(eng, lambda e: e.tensor_scalar(out=out, in0=in0, scalar1=s1, scalar2=s2, op0=op0, op1=op1), reads, writes)

        def CP(out, in_, reads, writes, eng="dve"):
            P.op(eng, lambda e: e.tensor_copy(out=out, in_=in_), reads, writes)

        def MEMSET(ap, val, writes, eng="dve"):
            P.op(eng, lambda e: e.memset(ap, val), (), writes)

        def DMA(q, out, in_, reads, writes):
            P.dma(q, lambda e: e.dma_start(out=out, in_=in_), reads, writes)

        def gcol(l, kind, c):
            i = ((l * 4 + kind) * 8 + c) if l < L_ALL else (L_ALL * 4 * 8 + c)
            return gains[:, i:i + 1]

        DMA("sp", gains[:], I["gains"], (), [B_const])
        DMA("sp", scw[:], I["scw"], (), [B_const])
        DMA("sp", fcw[:], I["fcw"], (), [B_const])
        DMA("sp", wg[:], I["wg_aug"], (), [B_const])
        DMA("sp", gn[:], I["gn"], (), [B_const])
        DMA("sp", hm[:], I["hm"], (), [B_const])
        DMA("pool", ident[:], I["ident"], (), [B_const])
        MEMSET(ones_bf[:], 1.0, [B_const])
        MEMSET(ones_f[:], 1.0, [B_const])
        P.flush()

        def rmsnorm_tile(xt, Bx, ntok, l, kind, sq, Bsq, rstd, Brstd, hout, Bh, rot):
            ACT(sq[:, :, 0:ntok], xt[:, :, 0:ntok], AF.Square, [Bx], [Bsq])
            bk, Bb = rot.next()
            for c in range(8):
                MM(bk[:, 0:ntok], ones_bf[:], sq[:, c, 0:ntok], c == 0, c == 7, [Bsq, B_const], [Bb])
            ACT(rstd[:, 0:ntok], bk[:, 0:ntok], AF.Ln, [Bb], [Brstd], scale=1.0 / D, bias=EPS)
            ACT(rstd[:, 0:ntok], rstd[:, 0:ntok], AF.Exp, [Brstd], [Brstd], scale=-0.5)
            for c in range(8):
                STT(hout[:, c, 0:ntok], xt[:, c, 0:ntok], gcol(l, kind, c), rstd[:, 0:ntok], ALU.mult, ALU.mult,
                    [Bx, Brstd, B_const], [Bh[c]])

        def softmax_norm(po, Bpo, ydst, By, es_bufs, rot_bc):
            lnd, rden, bc_sb, Bn = es_bufs
            ACT(lnd[64:65, :], po[64:65, :], AF.Ln, [Bpo], [Bn[0]])
            ACT(rden[64:65, :], lnd[64:65, :], AF.Exp, [Bn[0]], [Bn[1]], scale=-1.0)
            bk, Bb = rot_bc.next()
            MM(bk[0:64, :], ones_f[64:65, 0:64], rden[64:65, :], True, True, [Bn[1], B_const], [Bb])
            ACT(bc_sb[0:64, :], bk[0:64, :], AF.Copy, [Bb], [Bn[2]])
            TT(ydst, po[0:64, :], bc_sb[0:64, :], ALU.mult, [Bpo, Bn[2]], By)

        for l in range(n_layers):
            xsrc = (lambda c, t0, n: I["xT"][c * 128:(c + 1) * 128, t0:t0 + n]) if l == 0 else None
            with ExitStack() as es:
                win = sbt(es, "win", [128, 8, 3088], BF16)
                B_win = Buf("win")
                xt2 = [sbt(es, "xt%d" % i, [128, 8, 512], F32) for i in range(2)]
                B_xt2 = bufs(2, "xt")
                sq = sbt(es, "sq", [128, 8, 512], BF16); B_sq = Buf("sq")
                rstd = sbt(es, "rstd", [128, 512], F32); B_rstd = Buf("rstd")
                hh2 = [sbt(es, "hh%d" % i, [128, 8, 512], BF16) for i in range(2)]; B_hh2 = [bufs(8, "hh%d_" % i) for i in range(2)]
                hh = hh2[0]; B_hh = B_hh2[0]
                tmpc = sbt(es, "tmpc", [128, 512], F32); B_tmpc = Buf("tmpc")
                ubuf = [sbt(es, "ubuf%d" % i, [128, 514], F32) for i in range(2)]; B_ubuf = bufs(2, "ubuf")
                c1 = sbt(es, "c1", [128, 512], F32); B_c1 = Buf("c1")
                ytile = sbt(es, "ytile", [128, 2, 512], BF16); B_ytile = bufs(2, "ytile")
                qst = sbt(es, "qst", [128, 4, 512], BF16); B_qst = bufs(4, "qst")
                kst = sbt(es, "kst", [128, 4, 512], BF16); B_kst = bufs(4, "kst")
                vst = sbt(es, "vst", [128, 4, 512], BF16); B_vst = bufs(4, "vst")
                vgst = sbt(es, "vgst", [128, 4, 256], BF16); B_vgst = bufs(4, "vgst")
                kendst = sbt(es, "kendst", [128, 4, 640], BF16); B_kendst = bufs(4, "kendst")
                sgst = sbt(es, "sgst", [128, 2, 512], BF16); B_sgst = bufs(2, "sgst")
                glr = sbt(es, "glr", [32, 512], F32); B_glr = Buf("glr")
                e1 = sbt(es, "e1", [128, 128], F32); B_e1 = Buf("e1")
                spl = sbt(es, "spl", [128, 128], F32); B_spl = Buf("spl")
                E1 = sbt(es, "E1", [128, 512], F32); B_E1 = bufs(4, "E1")
                E2 = sbt(es, "E2", [128, 512], F32); B_E2 = bufs(4, "E2")
                E3 = sbt(es, "E3", [128, 128], F32); B_E3 = Buf("E3")
                ltri = sbt(es, "ltri", [128, 128], F32)
                lrev = sbt(es, "lrev", [128, 128], F32)
                qdst = sbt(es, "qdst", [128, 512], BF16); B_qdst = Buf("qdst")
                kdst = sbt(es, "kdst", [128, 512], BF16); B_kdst = Buf("kdst")
                B_c = Buf("p1const")
                rot = Rot([0, 1, 2, 3, 4, 5])
                rot_hold = Rot([6, 7])

                win_src = I["w_in"][l].rearrange("(c p) n -> p c n", p=128)
                B_winh = bufs(2, "winh")
                for hi, (a, b) in enumerate([(0, 1544), (1544, 3088)]):
                    DMA("pool", win[:, :, a:b], win_src[:, :, a:b], (), [B_winh[hi]])

                def winb(c0, c1):
                    r = []
                    if c0 < 1544:
                        r.append(B_winh[0])
                    if c1 > 1544:
                        r.append(B_winh[1])
                    return r
                DMA("sp", ltri[:], I["ltri"], (), [B_c])
                DMA("sp", lrev[:], I["lrev"], (), [B_c])
                MEMSET(ubuf[0][:, 0:2], 0.0, [B_ubuf[0]])
                MEMSET(ubuf[1][:, 0:2], 0.0, [B_ubuf[1]])
                MEMSET(glr[:], 1.0, [B_glr])
                MEMSET(kendst[:], 0.0, B_kendst)
                scl = lambda ch, k: scw[:, (l * 2 + ch) * 4 + k:(l * 2 + ch) * 4 + k + 1]

                def proj(out, cols, rows_rhs, reads_extra, Bb, M=None):
                    for c in range(8):
                        MM(out, win[:, c, cols[0]:cols[1]], hh[:, c, :], c == 0, c == 7, winb(cols[0], cols[1]) + [B_hh[c]], [Bb])

                def load_norm(tt):
                    t0 = tt * 512
                    xt = xt2[tt % 2]; Bx = B_xt2[tt % 2]
                    if l == 0:
                        DMA("sp", xt[:], I["xT"].rearrange("(c p) t -> p c t", p=128)[:, :, t0:t0 + 512], (), [Bx])
                    else:
                        DMA("sp", xt[:], xs.rearrange("c p t -> p c t")[:, :, t0:t0 + 512], [B_xs[tt]], [Bx])
                    rmsnorm_tile(xt, Bx, 512, l, 0, sq, B_sq, rstd, B_rstd, hh2[tt % 2], B_hh2[tt % 2], rot)

                load_norm(0)
                for tt in range(8):
                    t0 = tt * 512
                    hh = hh2[tt % 2]; B_hh = B_hh2[tt % 2]
                    for ch in range(2):
                        pc, Bpc = rot.next()
                        proj(pc[:], (256 + 128 * ch, 384 + 128 * ch), None, None, Bpc)
                        ACT(tmpc[:], pc[:], AF.Copy, [Bpc], [B_tmpc])
                        ph, Bph = rot.next()
                        proj(ph[:], (512 + 128 * ch, 640 + 128 * ch), None, None, Bph)
                        TT(ubuf[ch][:, 2:514], ph[:], tmpc[:], ALU.mult, [Bph, B_tmpc], [B_ubuf[ch]])
                        ACT(c1[:], ubuf[ch][:, 2:514], AF.Identity, [B_ubuf[ch], B_const], [B_c1], scale=scl(ch, 2), bias=scl(ch, 3))
                        STT(c1[:], ubuf[ch][:, 1:513], scl(ch, 1), c1[:], ALU.mult, ALU.add, [B_ubuf[ch], B_c1, B_const], [B_c1])
                        STT(c1[:], ubuf[ch][:, 0:512], scl(ch, 0), c1[:], ALU.mult, ALU.add, [B_ubuf[ch], B_c1, B_const], [B_c1])
                        CP(ubuf[ch][:, 0:2], ubuf[ch][:, 512:514], [B_ubuf[ch]], [B_ubuf[ch]])
                        pb, Bpb = rot.next()
                        proj(pb[:], (128 * ch, 128 * ch + 128), None, None, Bpb)
                        TT(ytile[:, ch, :], pb[:], c1[:], ALU.mult, [Bpb, B_c1], [B_ytile[ch]])
                    DMA("pool", yconv_d.rearrange("c p t -> p c t")[:, :, t0:t0 + 512], ytile[:], B_ytile, [B_yconv[tt]])
                    for hp in range(4):
                        pq, Bpq = rot.next()
                        proj(pq[:], (768 + 128 * hp, 896 + 128 * hp), None, None, Bpq)
                        ACT(qst[:, hp, :], pq[:], AF.Identity, [Bpq], [B_qst[hp]], scale=0.125)
                        for two in range(2):
                            h = 2 * hp + two
                            DMA("pool", q_d[h][:, t0:t0 + 512], qst[64 * two:64 * two + 64, hp, :], [B_qst[hp]], [B_q[h][tt]])
                        pk, Bpk = rot.next()
                        proj(pk[:], (1280 + 128 * hp, 1408 + 128 * hp), None, None, Bpk)
                        CP(kst[:, hp, :], pk[:], [Bpk], [B_kst[hp]])
                        for two in range(2):
                            h = 2 * hp + two
                            DMA("pool", k_d[h][:, t0:t0 + 512], kst[64 * two:64 * two + 64, hp, :], [B_kst[hp]], [B_k[h][tt]])
                    if tt + 1 < 8:
                        load_norm(tt + 1)
                    for sub in range(4):
                        pv, Bpv = rot.next()
                        for c in range(8):
                            MM(pv[:], hh[:, c, sub * 128:(sub + 1) * 128], win[:, c, 1792:2304], c == 0, c == 7,
                               [B_winh[1], B_hh[c]], [Bpv])
                        if sub % 2 == 0:
                            ACT(vst[:, sub, :], pv[:], AF.Copy, [Bpv], [B_vst[sub]])
                        else:
                            CP(vst[:, sub, :], pv[:], [Bpv], [B_vst[sub]])
                        DMA("pool", v_d[tt * 4 + sub], vst[:, sub, :], [B_vst[sub]], [B_v[tt * 4 + sub]])
                    pgq, Bpgq = rot_hold.next()
                    proj(pgq[:], (2304, 2432), None, None, Bpgq)
                    pgk, Bpgk = rot_hold.next()
                    proj(pgk[:], (2432, 2560), None, None, Bpgk)
                    plr, Bplr = rot.next()
                    proj(plr[0:16, :], (3072, 3088), None, None, Bplr)
                    ACT(glr[0:16, :], plr[0:16, :], AF.Copy, [Bplr], [B_glr])
                    for sub in range(4):
                        kt = tt * 4 + sub
                        pkv, Bpkv = rot.next()
                        for c in range(8):
                            MM(pkv[:, 0:384], hh[:, c, sub * 128:(sub + 1) * 128], win[:, c, 2432:2816], c == 0, c == 7,
                               [B_winh[1], B_hh[c]], [Bpkv])
                        CP(vgst[:, sub, :], pkv[:, 128:384], [Bpkv], [B_vgst[sub]])
                        DMA("pool", vg_d[kt], vgst[:, sub, :], [B_vgst[sub]], [B_vg[kt]])
                        pla, Bpla = rot.next()
                        MM(pla[:, 0:128], glr[0:32, sub * 128:(sub + 1) * 128], wg[0:32, l * 128:(l + 1) * 128], True, True,
                           [B_glr, B_const], [Bpla])
                        ACT(e1[:], pla[:, 0:128], AF.Exp, [Bpla], [B_e1], scale=-1.0)
                        ACT(spl[:], e1[:], AF.Ln, [B_e1], [B_spl], bias=1.0)
                        pbt, Bpbt = rot.next()
                        MM(pbt[:, 0:128], spl[:], ltri[:], True, True, [B_spl, B_c], [Bpbt])
                        MM(pbt[:, 128:256], lrev[:], spl[:], True, True, [B_spl, B_c], [Bpbt])
                        ACT(E1[:, sub * 128:(sub + 1) * 128], pbt[:, 0:128], AF.Exp, [Bpbt], [B_E1[sub]])
                        ACT(E2[:, sub * 128:(sub + 1) * 128], pbt[:, 0:128], AF.Exp, [Bpbt], [B_E2[sub]], scale=-1.0)
                        ACT(E3[:], pbt[:, 128:256], AF.Exp, [Bpbt], [B_E3])
                        TT(kendst[:, sub, :].rearrange("p (h x) -> p h x", x=160)[:, :, 0:32],
                           pkv[:, 0:128].rearrange("p (h x) -> p h x", x=32),
                           E3[:].rearrange("p (h x) -> p h x", x=32), ALU.mult, [Bpkv, B_E3], [B_kendst[sub]])
                        DMA("pool", kend_d[kt], kendst[:, sub, 0:512], [B_kendst[sub]], [B_kend[kt]])
                        CP(dec[:, 2 * kt:2 * kt + 2], E1[:, sub * 128 + 63:sub * 128 + 128:64], [B_E1[sub]], [B_dec])
                    STT(qdst[:], pgq[:], 32.0 ** -0.5, E1[:], ALU.mult, ALU.mult, [Bpgq] + B_E1, [B_qdst])
                    DMA("pool", qd_d[:, t0:t0 + 512], qdst[:], [B_qdst], [B_qd[tt]])
                    TT(kdst[:], pgk[:], E2[:], ALU.mult, [Bpgk] + B_E2, [B_kdst])
                    DMA("pool", kd_d[:, t0:t0 + 512], kdst[:], [B_kdst], [B_kd[tt]])
                    for hp in range(2):
                        pgo, Bpgo = rot.next()
                        proj(pgo[:], (2816 + 128 * hp, 2944 + 128 * hp), None, None, Bpgo)
                        ACT(sgst[:, hp, :], pgo[:], AF.Silu, [Bpgo], [B_sgst[hp]])
                    DMA("pool", sg_d.rearrange("(hp two) p t -> (two p) hp t", two=2)[:, :, t0:t0 + 512], sgst[:], B_sgst, [B_sg[tt]])
                P.flush()

            if max_phase < 2:
                break
            with ExitStack() as es:
                QA = [sbt(es, "QA%d" % i, [96, T], BF16) for i in range(2)]
                KA = [sbt(es, "KA%d" % i, [96, T], BF16) for i in range(2)]
                VA = [sbt(es, "VA%d" % i, [128, 32, 65], BF16) for i in range(2)]
                B_QAm = bufs(2, "QAm"); B_KA = bufs(2, "KA"); B_VA = bufs(2, "VA")
                B_QAmask = [bufs(8, "QAmask%d_" % i) for i in range(2)]
                cm = sbt(es, "cm", [128, 4, 512], BF16)
                pmt = sbt(es, "pmt", [128, 32, 16], F32)
                ownfix = sbt(es, "ownfix", [128, 32, 16], F32)
                alibase = sbt(es, "alibase", [128, 32, 16], F32)
                B_c = Buf("p2const")
                km32 = sbt(es, "km32", [64, 16], F32); B_km32 = Buf("km32")
                kmb2 = [sbt(es, "kmb%d" % i, [64, 16], BF16) for i in range(2)]; B_kmb2 = bufs(2, "kmb")
                gm = sbt(es, "gm", [128, 4, 16], F32); B_gm = Buf("gm")
                top8 = sbt(es, "top8", [128, 4, 8], F32); B_top8 = Buf("top8")
                sel = sbt(es, "sel", [128, 4, 16], F32); B_sel = Buf("sel")
                stage = sbt(es, "stage", [128, 4, 128], BF16); B_stage = Buf("stage")
                pt = [sbt(es, "pt%d" % i, [128, 512], BF16) for i in range(8)]; B_pt = bufs(8, "pt")
                lnd = sbt(es, "lnd", [128, 512], F32)
                rden = sbt(es, "rden", [128, 512], F32)
                bc_sb = sbt(es, "bc_sb", [64, 512], F32)
                B_n = bufs(3, "nrm")
                yst = [sbt(es, "yst%d" % i, [64, 512], BF16) for i in range(2)]; B_yst = bufs(2, "yst")
                rot_s = Rot([0, 1, 2, 3, 6])
                rot_o = Rot([4, 5])
                rot_g = Rot([7])
                rot_t = Rot([7])

                DMA("pool", cm[:].rearrange("p a b -> p (a b)"), I["cm"], (), [B_c])
                DMA("sp", pmt[:].rearrange("p a b -> p (a b)"), I["pm"], (), [B_c])
                DMA("sp", ownfix[:].rearrange("p a b -> p (a b)"), I["ownfix"], (), [B_c])
                DMA("sp", alibase[:].rearrange("p a b -> p (a b)"), I["alibase"], (), [B_c])
                MEMSET(stage[:], 0.0, [B_stage])
                for i in range(2):
                    for hf in range(2):
                        DMA("pool", QA[i][80:96, hf * 2048:(hf + 1) * 2048], I["qstat"][:, hf * 2048:(hf + 1) * 2048], (), [B_QAm[i]])
                    MEMSET(VA[i][:, :, 64:65], 1.0, [B_VA[i]])
                ycnt = [0]

                def prologue(h):
                    bi = h % 2
                    DMA("sp", QA[bi][0:64, :], q_d[h], B_q[h], [B_QAm[bi]])
                    DMA("sp", KA[bi][0:64, :], k_d[h], B_k[h], [B_KA[bi]])
                    for hf in range(2):
                        DMA("pool", KA[bi][64:96, hf * 2048:(hf + 1) * 2048], I["kstat"][h][:, hf * 2048:(hf + 1) * 2048], (), [B_KA[bi]])
                    DMA("sp", VA[bi][:, :, 0:64], v_d.rearrange("k p (h e) -> p k h e", e=64)[:, :, h, :], B_v, [B_VA[bi]])

                def kmean(h):
                    bi = h % 2
                    P.op("dve", lambda e, bi=bi: e.tensor_reduce(out=km32[:], in_=KA[bi][0:64, :].rearrange("p (j k) -> p j k", k=256),
                                                                axis=AX.X, op=ALU.add), [B_KA[bi]], [B_km32])
                    ACT(kmb2[bi][:], km32[:], AF.Identity, [B_km32], [B_kmb2[bi]], scale=1.0 / 256)

                def gate_mm(h, g):
                    bi = h % 2
                    slope = 2.0 ** (-(h + 1))
                    pg, Bpg = rot_g.next()
                    for qt in range(4):
                        q0 = g * 512 + qt * 128
                        MM(pg[:, qt * 16:(qt + 1) * 16], QA[bi][0:64, q0:q0 + 128], kmb2[bi][:], True, True,
                           [B_QAm[bi], B_kmb2[bi]], [Bpg])
                    TT(gm[:], pg[:, 0:64].rearrange("p (a b) -> p a b", b=16), pmt[:, 4 * g:4 * g + 4, :], ALU.add,
                       [Bpg, B_c], [B_gm])
                    for qt in range(4):
                        P.op("dve", lambda e, qt=qt: e.max(out=top8[:, qt, :], in_=gm[:, qt, :]), [B_gm], [B_top8])
                    for qt in range(4):
                        TS(sel[:, qt, :], gm[:, qt, :], top8[:, qt, 2:3], None, ALU.is_ge, None, [B_gm, B_top8], [B_sel])
                    TS(sel[:], sel[:], -1.0, -NEG, ALU.add, ALU.mult, [B_sel], [B_sel])
                    TT(sel[:], sel[:], ownfix[:, 4 * g:4 * g + 4, :], ALU.max, [B_sel, B_c], [B_sel])
                    STT(stage[:, :, 64:80], alibase[:, 4 * g:4 * g + 4, :], slope, sel[:], ALU.mult, ALU.add,
                        [B_sel, B_c], [B_stage])

                def gate_T(h, g):
                    bi = h % 2
                    pT, BpT = rot_t.next()
                    for qt in range(4):
                        MM(pT[:, qt * 128:(qt + 1) * 128], stage[:, qt, :], ident[:], True, True, [B_stage, B_const], [BpT])
                    ACT(QA[bi][64:80, g * 512:(g + 1) * 512], pT[64:80, :], AF.Copy, [BpT], [B_QAmask[bi][g]])

                NPT = len(pt)
                LOOK = 4

                def attention(h, g):
                    bi = h % 2
                    nkt = 4 * (g + 1)
                    po, Bpo = rot_o.next()
                    qs = QA[bi][0:96, g * 512:(g + 1) * 512]

                    def S_tile(kt):
                        pS, BpS = rot_s.next()
                        diag = kt >= 4 * g
                        MM(pS[:], KA[bi][0:96, kt * 128:(kt + 1) * 128], qs, True, not diag,
                           [B_KA[bi], B_QAm[bi], B_QAmask[bi][g]], [BpS])
                        if diag:
                            MM(pS[:], ident[:], cm[:, kt - 4 * g, :], False, True, [B_const, B_c], [BpS])
                        j = kt % NPT
                        ACT(pt[j][:], pS[:], AF.Exp, [BpS], [B_pt[j]])

                    def PV_tile(kt):
                        j = kt % NPT
                        MM(po[0:65, :], VA[bi][:, kt, 0:65], pt[j][:], kt == 0, kt == nkt - 1, [B_VA[bi], B_pt[j]], [Bpo])
                    for kt in range(min(LOOK, nkt)):
                        S_tile(kt)
                    for kt in range(nkt):
                        if kt + LOOK < nkt:
                            S_tile(kt + LOOK)
                        PV_tile(kt)
                    yi = ycnt[0] % 2
                    ycnt[0] += 1
                    softmax_norm(po, Bpo, yst[yi][:], [B_yst[yi]], (lnd, rden, bc_sb, B_n), rot_t)
                    DMA("pool", ymoba_d[h][:, g * 512:(g + 1) * 512], yst[yi][:], [B_yst[yi]], [B_ymoba[h][g]])

                prologue(0)
                kmean(0)
                for g in range(8):
                    gate_mm(0, g)
                    gate_T(0, g)
                pending = []
                for h in range(8):
                    if h + 1 < 8:
                        prologue(h + 1)
                    for g in range(8):
                        if g == 5 and h + 1 < 8:
                            kmean(h + 1)
                            pending.extend((h + 1, gg) for gg in range(8))
                        task = pending.pop(0) if pending else None
                        if task:
                            gate_mm(*task)
                        attention(h, g)
                        if task:
                            gate_T(*task)
                assert not pending
                qdT = sbt(es, "qdT", [128, T], BF16); B_qdT = Buf("qdT")
                kdT = sbt(es, "kdT", [128, T], BF16); B_kdT = Buf("kdT")
                kend = sbt(es, "kend", [128, 32, 512], BF16); B_kendS = Buf("kendS")
                vg = sbt(es, "vg", [128, 32, 256], BF16); B_vgS = Buf("vgS")
                sg = sbt(es, "sg", [64, 4, T], BF16); B_sgS = Buf("sgS")
                Sall = sbt(es, "Sall", [128, 65, 64], F32); B_S = bufs(65, "S")
                Sbf = sbt(es, "Sbf", [128, 64, 64], BF16); B_Sbf = Buf("Sbf")
                tri01 = sbt(es, "tri01", [128, 512], BF16); B_c = Buf("p3const")
                qbd = [sbt(es, "qbd%d" % i, [128, 4, 128], BF16) for i in range(2)]; B_qbd = bufs(2, "qbd")
                Am = [sbt(es, "Am%d" % i, [128, 4, 128], BF16) for i in range(2)]; B_Am = bufs(2, "Am")
                sqg = sbt(es, "sqg", [64, 512], BF16); B_sqg = Buf("sqg")
                rs = sbt(es, "rs", [64, 512], F32); B_rs = Buf("rs")
                t1 = sbt(es, "t1", [64, 512], F32); B_t1 = Buf("t1")
                ygst = [sbt(es, "ygst%d" % i, [64, 4, 128], BF16) for i in range(2)]; B_ygst = bufs(2, "ygst")
                rot_u = Rot([0, 1])
                rot_a = Rot([2, 3])
                rot_o = Rot([4, 5])
                rot_s = Rot([6, 7])

                DMA("pool", tri01[:], I["tri01"], (), [B_c])
                DMA("sp", qdT[:], qd_d, B_qd, [B_qdT])
                DMA("sp", kdT[:], kd_d, B_kd, [B_kdT])
                for part in range(4):
                    DMA("sp", kend[:, part * 8:(part + 1) * 8, :], kend_d.rearrange("k p x -> p k x")[:, part * 8:(part + 1) * 8, :],
                        B_kend[part * 8:(part + 1) * 8], [B_kendS])
                    DMA("sp", vg[:, part * 8:(part + 1) * 8, :], vg_d.rearrange("k p x -> p k x")[:, part * 8:(part + 1) * 8, :],
                        B_vg[part * 8:(part + 1) * 8], [B_vgS])
                DMA("sp", sg[:], sg_d.rearrange("h p t -> p h t"), B_sg, [B_sgS])
                MEMSET(Sall[:, 0, :], 0.0, [B_S[0]])
                rot_ue = Rot([0, 1])
                rot_uo = Rot([6, 7])
                for grp in range(4):
                    pUe, BpUe = rot_ue.next()
                    pUo, BpUo = rot_uo.next()
                    for n in range(16):
                        N = grp * 16 + n
                        kt = N // 2
                        p0 = 64 * (N % 2)
                        pU, BpU = (pUe, BpUe) if N % 2 == 0 else (pUo, BpUo)
                        cs = slice((n // 2) * 64, (n // 2) * 64 + 64)
                        for h in range(4):
                            MM(pU[:, cs], kend[p0:p0 + 64, kt, h * 128:(h + 1) * 128],
                               vg[p0:p0 + 64, kt, h * 64:(h + 1) * 64], h == 0, h == 3, [B_kendS, B_vgS], [BpU])
                    for n in range(16):
                        N = grp * 16 + n
                        pU, BpU = (pUe, BpUe) if N % 2 == 0 else (pUo, BpUo)
                        cs = slice((n // 2) * 64, (n // 2) * 64 + 64)
                        STT(Sall[:, N + 1, :], Sall[:, N, :], dec[:, N:N + 1], pU[:, cs], ALU.mult, ALU.add,
                            [B_S[N], B_dec, BpU], [B_S[N + 1]])
                ACT(Sbf[:].rearrange("p a b -> p (a b)"), Sall[:, 0:64, :].rearrange("p a b -> p (a b)"), AF.Copy, B_S[0:64], [B_Sbf])
                for kt in range(32):
                    i2 = kt % 2
                    tok = slice(kt * 128, (kt + 1) * 128)
                    for h in range(4):
                        TS(qbd[i2][:, h, :], qdT[:, tok], hm[:, h:h + 1], None, ALU.mult, None, [B_qdT, B_const], [B_qbd[i2]])
                    pA, BpA = rot_a.next()
                    MM(pA[:], kdT[:, tok], qbd[i2][:].rearrange("p a b -> p (a b)"), True, True, [B_kdT, B_qbd[i2]], [BpA])
                    TT(Am[i2][:].rearrange("p a b -> p (a b)"), pA[:], tri01[:], ALU.mult, [BpA, B_c], [B_Am[i2]])
                    po, Bpo = rot_o.next()
                    pov = po[0:64, :].rearrange("p (h c) -> p h c", c=128)
                    for half in range(2):
                        MM(pov[:, :, 64 * half:64 * half + 64], Sbf[:, 2 * kt + half, :], qbd[i2][:, :, 64 * half:64 * half + 64],
                           half == 0, False, [B_Sbf, B_qbd[i2]], [Bpo], skip_group_check=True)
                    for h in range(4):
                        MM(po[0:64, h * 128:(h + 1) * 128], vg[:, kt, h * 64:(h + 1) * 64], Am[i2][:, h, :], False, h == 3,
                           [B_vgS, B_Am[i2]], [Bpo], skip_group_check=True)
                    ACT(sqg[:], po[0:64, :], AF.Square, [Bpo], [B_sqg])
                    pss, Bpss = rot_s.next()
                    MM(pss[0:64, :], ones_bf[0:64, 0:64], sqg[:], True, True, [B_sqg, B_const], [Bpss])
                    ACT(rs[:], pss[0:64, :], AF.Ln, [Bpss], [B_rs], scale=1.0 / 64, bias=EPS)
                    ACT(rs[:], rs[:], AF.Exp, [B_rs], [B_rs], scale=-0.5)
                    STT(t1[:], po[0:64, :], gn[:, l:l + 1], rs[:], ALU.mult, ALU.mult, [Bpo, B_rs, B_const], [B_t1])
                    TT(ygst[i2][:], t1[:].rearrange("p (h c) -> p h c", c=128), sg[:, :, tok], ALU.mult, [B_t1, B_sgS], [B_ygst[i2]])
                    DMA("pool", ygla_d.rearrange("h p t -> p h t")[:, :, tok], ygst[i2][:], [B_ygst[i2]], [B_ygla[kt]])
                P.flush()

            if max_phase < 4:
                if debug:
                    DMA("sp", dbg["yconv"], yconv_d, B_yconv, [Buf()])
                    DMA("sp", dbg["ymoba"], ymoba_d, [b for hb in B_ymoba for b in hb], [Buf()])
                    DMA("sp", dbg["ygla"], ygla_d, B_ygla, [Buf()])
                    P.flush()
                break
            with ExitStack() as es:
                wo_c = sbt(es, "wo_c", [128, 2, D], BF16)
                wo_m = sbt(es, "wo_m", [128, 4, D], BF16)
                wo_g = sbt(es, "wo_g", [128, 2, D], BF16)
                wq = sbt(es, "wq", [128, 8, 256], BF16)
                wkv = sbt(es, "wkv", [128, 8, 512], BF16)
                wo_x = sbt(es, "wo_x", [128, 4, D], BF16)
                B_w = Buf("p4w")
                memt = sbt(es, "memt", [128, 8, 256], F32); B_memt = Buf("memt")
                memn = sbt(es, "memn", [128, 8, 256], BF16); B_memn = bufs(8, "memn")
                KmT = sbt(es, "KmT", [128, 4, 256], BF16); B_KmT = Buf("KmT")
                Vm = sbt(es, "Vm", [128, 2, 4, 65], BF16); B_Vm = Buf("Vm")
                xt2 = [sbt(es, "xt%d" % i, [128, 8, 512], F32) for i in range(2)]; B_xt2 = [bufs(8, "xt%d_" % i) for i in range(2)]
                B_xtall = bufs(2, "xtall")
                yc = sbt(es, "yc", [128, 2, 512], BF16); B_yc = Buf("yc")
                ym = sbt(es, "ym", [128, 4, 512], BF16); B_ym = Buf("ym")
                yg = sbt(es, "yg", [128, 2, 512], BF16); B_yg = Buf("yg")
                sq = sbt(es, "sq", [128, 8, 512], BF16); B_sq = Buf("sq")
                rstd = sbt(es, "rstd", [128, 512], F32); B_rstd = Buf("rstd")
                hh = sbt(es, "hh", [128, 8, 512], BF16); B_hh = bufs(8, "hh")
                qx4 = [sbt(es, "qx%d" % i, [128, 512], BF16) for i in range(4)]; B_qx4 = bufs(4, "qx")
                pt8 = [sbt(es, "pt%d" % i, [128, 512], BF16) for i in range(8)]; B_pt8 = bufs(8, "pt")
                ox = sbt(es, "ox", [128, 4, 512], BF16); B_ox = bufs(4, "ox")
                nset = [(sbt(es, "lnd%d" % i, [128, 512], F32), sbt(es, "rden%d" % i, [128, 512], F32),
                         sbt(es, "bc_sb%d" % i, [64, 512], F32), bufs(3, "nrm%d_" % i)) for i in range(2)]
                rot = Rot([0, 1, 2, 3, 4])
                rot_o = Rot([5, 6])
                rot_t = Rot([7])

                wsrc = I["w_out"][l]
                DMA("pool", wo_c[:], wsrc[0:256].rearrange("(c p) n -> p c n", p=128), (), [B_w])
                DMA("pool", wo_m[:], wsrc[256:768].rearrange("(h p) n -> p h n", p=128), (), [B_w])
                DMA("pool", wo_g[:], wsrc[768:1024].rearrange("(h p) n -> p h n", p=128), (), [B_w])
                DMA("pool", wq[:], I["xattn_wq"][l].rearrange("(c p) n -> p c n", p=128), (), [B_w])
                DMA("pool", wkv[:], I["xattn_wkv"][l].rearrange("(c p) n -> p c n", p=128), (), [B_w])
                MEMSET(wo_x[64:128, :, :], 0.0, [B_w])
                MEMSET(KmT[64:128, :, :], 0.0, [B_KmT])
                MEMSET(ox[64:128, :, :], 0.0, B_ox)
                for i in range(4):
                    MEMSET(qx4[i][64:128, :], 0.0, [B_qx4[i]])
                DMA("pool", wo_x[0:64, :, :], I["xattn_wo"][l].rearrange("(h p) n -> p h n", p=64), (), [B_w])
                DMA("sp", memt[:], I["memT"].rearrange("(c p) t -> p c t", p=128), (), [B_memt])
                MEMSET(Vm[:, :, :, 64:65], 1.0, [B_Vm])
                rmsnorm_tile(memt, B_memt, 256, l, 2, sq, B_sq, rstd, B_rstd, memn, B_memn, rot)
                for h in range(4):
                    pk, Bpk = rot.next()
                    for c in range(8):
                        MM(pk[0:64, 0:256], wkv[:, c, 64 * h:64 * h + 64], memn[:, c, :], c == 0, c == 7, [B_w, B_memn[c]], [Bpk])
                    CP(KmT[0:64, h, :], pk[0:64, 0:256], [Bpk], [B_KmT])
                for mt in range(2):
                    pv, Bpv = rot.next()
                    for c in range(8):
                        MM(pv[:, 0:256], memn[:, c, mt * 128:(mt + 1) * 128], wkv[:, c, 256:512], c == 0, c == 7, [B_w, B_memn[c]], [Bpv])
                    CP(Vm[:, mt, :, 0:64], pv[:, 0:256].rearrange("p (h e) -> p h e", e=64), [Bpv], [B_Vm])

                def Wst(tt):
                    t0 = tt * 512
                    xi = tt % 2
                    xt = xt2[xi]; Bx = B_xt2[xi]
                    if l == 0:
                        DMA("sp", xt[:], I["xT"].rearrange("(c p) t -> p c t", p=128)[:, :, t0:t0 + 512], (), Bx)
                    else:
                        DMA("sp", xt[:], xs.rearrange("c p t -> p c t")[:, :, t0:t0 + 512], [B_xs[tt]], Bx)
                    DMA("sp", yc[:], yconv_d.rearrange("c p t -> p c t")[:, :, t0:t0 + 512], [B_yconv[tt]], [B_yc])
                    DMA("sp", ym[:], ymoba_d.rearrange("(hp two) p t -> (two p) hp t", two=2)[:, :, t0:t0 + 512], [B_ymoba[h][tt] for h in range(8)], [B_ym])
                    DMA("sp", yg[:], ygla_d.rearrange("(hp two) p t -> (two p) hp t", two=2)[:, :, t0:t0 + 512], B_ygla[tt * 4:tt * 4 + 4], [B_yg])
                    for oc in range(8):
                        cs = slice(oc * 128, (oc + 1) * 128)
                        pw, Bpw = rot.next()
                        for c in range(2):
                            MM(pw[:], wo_c[:, c, cs], yc[:, c, :], c == 0, False, [B_w, B_yc], [Bpw])
                        for h in range(4):
                            MM(pw[:], wo_m[:, h, cs], ym[:, h, :], False, False, [B_w, B_ym], [Bpw])
                        for h in range(2):
                            MM(pw[:], wo_g[:, h, cs], yg[:, h, :], False, h == 1, [B_w, B_yg], [Bpw])
                        TT(xt[:, oc, :], pw[:], xt[:, oc, :], ALU.add, [Bpw, Bx[oc]], [Bx[oc]])

                def Nst(tt):
                    xi = tt % 2
                    xt = xt2[xi]; Bx = B_xt2[xi]
                    ACT(sq[:], xt[:], AF.Square, Bx, [B_sq])
                    bk, Bb = rot.next()
                    for c in range(8):
                        MM(bk[:], ones_bf[:], sq[:, c, :], c == 0, c == 7, [B_sq, B_const], [Bb])
                    ACT(rstd[:], bk[:], AF.Ln, [Bb], [B_rstd], scale=1.0 / D, bias=EPS)
                    ACT(rstd[:], rstd[:], AF.Exp, [B_rstd], [B_rstd], scale=-0.5)
                    for c in range(8):
                        STT(hh[:, c, :], xt[:, c, :], gcol(l, 1, c), rstd[:], ALU.mult, ALU.mult, [Bx[c], B_rstd, B_const], [B_hh[c]])

                def Xst(tt):
                    for h in range(4):
                        pq, Bpq = rot.next()
                        for c in range(8):
                            MM(pq[0:64, :], wq[:, c, 64 * h:64 * h + 64], hh[:, c, :], c == 0, c == 7, [B_w, B_hh[c]], [Bpq])
                        ACT(qx4[h][0:64, :], pq[0:64, :], AF.Identity, [Bpq], [B_qx4[h]], scale=0.125)
                    for h in range(4):
                        for mt in range(2):
                            pS, BpS = rot.next()
                            MM(pS[:], KmT[:, h, mt * 128:(mt + 1) * 128], qx4[h][:], True, True, [B_KmT, B_qx4[h]], [BpS])
                            ACT(pt8[h * 2 + mt][:], pS[:], AF.Exp, [BpS], [B_pt8[h * 2 + mt]])
                    for h in range(4):
                        po, Bpo = rot_o.next()
                        for mt in range(2):
                            MM(po[0:65, :], Vm[:, mt, h, 0:65], pt8[h * 2 + mt][:], mt == 0, mt == 1, [B_Vm, B_pt8[h * 2 + mt]], [Bpo])
                        softmax_norm(po, Bpo, ox[0:64, h, :], [B_ox[h]], nset[h % 2], rot_t)

                def Ost(tt):
                    t0 = tt * 512
                    xi = tt % 2
                    xt = xt2[xi]; Bx = B_xt2[xi]
                    for oc in range(8):
                        cs = slice(oc * 128, (oc + 1) * 128)
                        pw, Bpw = rot.next()
                        for h in range(4):
                            MM(pw[:], wo_x[:, h, cs], ox[:, h, :], h == 0, h == 3, [B_w, B_ox[h]], [Bpw])
                        TT(xt[:, oc, :], pw[:], xt[:, oc, :], ALU.add, [Bpw, Bx[oc]], [Bx[oc]])
                    DMA("pool", xs.rearrange("c p t -> p c t")[:, :, t0:t0 + 512], xt[:], Bx, [B_xs[tt]])

                Wst(0)
                for tt in range(8):
                    Nst(tt)
                    if tt + 1 < 8:
                        Wst(tt + 1)
                    Xst(tt)
                    Ost(tt)
                P.flush()

            if debug and l == 0:
                DMA("sp", dbg["yconv"], yconv_d, B_yconv, [Buf()])
                DMA("sp", dbg["ymoba"], ymoba_d, [b for hb in B_ymoba for b in hb], [Buf()])
                DMA("sp", dbg["ygla"], ygla_d, B_ygla, [Buf()])
                DMA("sp", dbg["x2"], xs, B_xs, [Buf()])
                P.flush()

            if max_phase < 6:
                break
            with ExitStack() as es:
                NTK = 256
                NT6 = T // NTK
                wup = sbt(es, "wup", [128, 8, 2 * DFF], BF16)
                wdn = sbt(es, "wdn", [128, 22, D], BF16)
                B_wup = [bufs(8, "wup%d_" % hf) for hf in range(2)]
                B_wdn = bufs(4, "wdn")
                xt2 = [sbt(es, "xt%d" % i, [128, 8, NTK], F32) for i in range(2)]; B_xt2 = [bufs(8, "xt%d_" % i) for i in range(2)]
                sq = sbt(es, "sq", [128, 8, NTK], BF16); B_sq = Buf("sq")
                rstd = sbt(es, "rstd", [128, NTK], F32); B_rstd = Buf("rstd")
                hh2 = [sbt(es, "hh%d" % i, [128, 8, NTK], BF16) for i in range(2)]; B_hh2 = [bufs(8, "hh%d_" % i) for i in range(2)]
                NB = 3
                ab = [sbt(es, "ab%d" % i, [128, 2, NTK + 2], F32) for i in range(NB)]; B_ab = bufs(NB, "ab")
                cc = [sbt(es, "cc%d" % i, [128, 2, NTK], F32) for i in range(NB)]; B_cc = [bufs(2, "cc%d_" % i) for i in range(NB)]
                sl = [sbt(es, "sl%d" % i, [128, NTK], F32) for i in range(NB)]; B_sl = bufs(NB, "sl")
                hst = sbt(es, "hst", [128, 22, 2, 2], F32); B_hst = bufs(22, "hst")
                hact = sbt(es, "hact", [128, 22, NTK], BF16); B_hact = bufs(22, "hact")
                rot = Rot([0, 1, 2, 3, 4, 5, 6, 7])

                up_src = I["ffn_w_up"][l].rearrange("(c p) n -> p c n", p=128)
                for grp4 in range(4):
                    for hf in range(2):
                        a = hf * DFF + grp4 * 688
                        DMA("pool", wup[:, :, a:a + 688], up_src[:, :, a:a + 688], (), [B_wup[hf][2 * grp4], B_wup[hf][2 * grp4 + 1]])
                dn_src = I["ffn_w_down"][l]
                for gi, jg in enumerate(range(0, 21, 7)):
                    DMA("pool", wdn[:, jg:jg + 7, :], dn_src[jg * 128:(jg + 7) * 128].rearrange("(j p) n -> p j n", p=128), (), [B_wdn[gi]])
                DMA("pool", wdn[0:64, 21, :], dn_src[2688:2752], (), [B_wdn[3]])
                MEMSET(hst[:].rearrange("p a b c -> p (a b c)"), 0.0, B_hst)
                fcl = lambda idx, k: fcw[:, (l * 44 + idx) * 4 + k:(l * 44 + idx) * 4 + k + 1]
                last = (l == n_layers - 1)

                def wup_bufs(hf, j, rows):
                    g0 = (j * 128) // 344
                    g1 = (j * 128 + rows - 1) // 344
                    return [B_wup[hf][g] for g in range(g0, g1 + 1)]

                def load_x(tt):
                    xi = tt % 2
                    DMA("sp", xt2[xi][:], xs.rearrange("c p t -> p c t")[:, :, tt * NTK:(tt + 1) * NTK], [B_xs[tt * NTK // 512]], B_xt2[xi])

                def norm_sq(tt):
                    ACT(sq[:], xt2[tt % 2][:], AF.Square, B_xt2[tt % 2], [B_sq])

                def norm_pe(tt, kind_l, kind):
                    bk, Bb = rot.next()
                    for c in range(8):
                        MM(bk[:, 0:NTK], ones_bf[:], sq[:, c, :], c == 0, c == 7, [B_sq, B_const], [Bb])
                    ACT(rstd[:], bk[:, 0:NTK], AF.Ln, [Bb], [B_rstd], scale=1.0 / D, bias=EPS)
                    ACT(rstd[:], rstd[:], AF.Exp, [B_rstd], [B_rstd], scale=-0.5)

                def norm_h(tt, c):
                    xi = tt % 2
                    STT(hh2[xi][:, c, :], xt2[xi][:, c, :], gcol(l, 3, c), rstd[:], ALU.mult, ALU.mult,
                        [B_xt2[xi][c], B_rstd, B_const], [B_hh2[xi][c]])

                load_x(0)
                norm_sq(0)
                norm_pe(0, l, 3)
                for c in range(8):
                    norm_h(0, c)
                for tt in range(NT6):
                    t0 = tt * NTK
                    xi = tt % 2
                    xt = xt2[xi]; Bx = B_xt2[xi]
                    hh = hh2[xi]; B_hh = B_hh2[xi]
                    pas = {}

                    def stageA(j):
                        rows = 128 if j < 21 else 64
                        i3 = j % NB
                        pa, Bpa = rot.next()
                        pas[j] = (pa, Bpa)
                        for half in range(2):
                            c0 = half * DFF + j * 128
                            wb = wup_bufs(half, j, rows)
                            for c in range(8):
                                MM(pa[0:rows, half * NTK:(half + 1) * NTK], wup[:, c, c0:c0 + rows], hh[:, c, :], c == 0, c == 7,
                                   wb + [B_hh[c]], [Bpa])
                        abt = ab[i3]
                        ACT(abt[0:rows, :, 2:NTK + 2], pa[0:rows, :].rearrange("p (a b) -> p a b", b=NTK), AF.Copy, [Bpa], [B_ab[i3]])
                        CP(abt[0:rows, :, 0:2], hst[0:rows, j, :, :], [B_hst[j]], [B_ab[i3]])

                    def stageB(j):
                        rows = 128 if j < 21 else 64
                        i3 = j % NB
                        abt = ab[i3]; cct = cc[i3]
                        for half in range(2):
                            idx = half * 22 + j
                            ACT(cct[0:rows, half, :], abt[0:rows, half, 2:NTK + 2], AF.Identity, [B_ab[i3], B_const], [B_cc[i3][half]],
                                scale=fcl(idx, 2)[0:rows], bias=fcl(idx, 3)[0:rows])
                        for k in (1, 0):
                            for half in range(2):
                                idx = half * 22 + j
                                STT(cct[0:rows, half, :], abt[0:rows, half, k:NTK + k], fcl(idx, k)[0:rows], cct[0:rows, half, :], ALU.mult, ALU.add,
                                    [B_ab[i3], B_cc[i3][half], B_const], [B_cc[i3][half]])
                        CP(hst[0:rows, j, :, :], abt[0:rows, :, NTK:NTK + 2], [B_ab[i3]], [B_hst[j]])

                    def stageC(j):
                        rows = 128 if j < 21 else 64
                        i3 = j % NB
                        cct = cc[i3]
                        ACT(sl[i3][0:rows, :], cct[0:rows, 0, :], AF.Silu, [B_cc[i3][0]], [B_sl[i3]])
                        TT(hact[0:rows, j, :], sl[i3][0:rows, :], cct[0:rows, 1, :], ALU.mult, [B_sl[i3], B_cc[i3][1]], [B_hact[j]])

                    for step in range(22 + 2):
                        if step < 22:
                            stageA(step)
                        if 0 <= step - 1 < 22:
                            stageB(step - 1)
                        if 0 <= step - 2 < 22:
                            stageC(step - 2)
                        if tt + 1 < NT6:
                            if step == 4:
                                load_x(tt + 1)
                            if step == 8:
                                norm_sq(tt + 1)
                            if step == 11:
                                norm_pe(tt + 1, l, 3)
                            if 13 <= step < 21:
                                norm_h(tt + 1, step - 13)
                    for oc in range(8):
                        cs = slice(oc * 128, (oc + 1) * 128)
                        pd, Bpd = rot.next()
                        for j in range(22):
                            rows = 128 if j < 21 else 64
                            MM(pd[:, 0:NTK], wdn[0:rows, j, cs], hact[0:rows, j, :], j == 0, j == 21, [B_wdn[min(j // 7, 3)], B_hact[j]], [Bpd])
                        TT(xt[:, oc, :], pd[:, 0:NTK], xt[:, oc, :], ALU.add, [Bpd, Bx[oc]], [Bx[oc]])
                    if not last:
                        DMA("pool", xs.rearrange("c p t -> p c t")[:, :, t0:t0 + NTK], xt[:], Bx, [B_xs[t0 // 512]])
                    else:
                        ACT(hact[:, 0:8, :], xt[:], AF.Square, Bx, B_hact[0:8])
                        bk, Bb = rot.next()
                        for c in range(8):
                            MM(bk[:, 0:NTK], ones_bf[:], hact[:, c, :], c == 0, c == 7, B_hact[0:8] + [B_const], [Bb])
                        ACT(sl[0][:], bk[:, 0:NTK], AF.Ln, [Bb], [B_sl[0]], scale=1.0 / D, bias=EPS)
                        ACT(sl[0][:], sl[0][:], AF.Exp, [B_sl[0]], [B_sl[0]], scale=-0.5)
                        for c in range(8):
                            STT(xt[:, c, :], xt[:, c, :], gcol(L_ALL, 0, c), sl[0][:], ALU.mult, ALU.mult, [Bx[c], B_sl[0], B_const], [Bx[c]])
                        DMA("pool", outT.rearrange("(c p) t -> p c t", p=128)[:, :, t0:t0 + NTK], xt[:], Bx, [Buf()])
                P.flush()
        build.ninstr = P.ninstr
    return nc


_CACHE = {}


def make_in_maps(inputs):
    consts = host_consts()
    params = host_params(inputs)
    shared = {}
    for nm in ["w_in", "w_out", "xattn_wq", "xattn_wkv", "xattn_wo", "ffn_w_up", "ffn_w_down"]:
        shared[nm] = np.ascontiguousarray(inputs[nm], dtype=np.float32)
    shared.update(params)
    shared.update(consts)
    in_maps = []
    for core in range(8):
        b = core % 4
        m = dict(shared)
        m["xT"] = np.ascontiguousarray(inputs["x"][b].T)
        m["memT"] = np.ascontiguousarray(inputs["mem"][b].T)
        in_maps.append(m)
    return in_maps


def kernel(**inputs):
    inputs = {k: np.asarray(v) for k, v in inputs.items()}
    if "nc" not in _CACHE:
        _CACHE["nc"] = build()
    nc = _CACHE["nc"]
    in_maps = make_in_maps(inputs)
    res = run_bass_kernel_spmd(nc, in_maps, core_ids=list(range(8)))
    out = np.stack([np.ascontiguousarray(res.results[b]["outT"].T) for b in range(4)], axis=0)
    return out.astype(np.float32)
```

```python
import numpy as np
from contextlib import ExitStack
import concourse.bass as bass
import concourse.mybir as mybir
from concourse.bass_utils import run_bass_kernel_spmd

F32 = mybir.dt.float32
BF16 = mybir.dt.bfloat16
AF = mybir.ActivationFunctionType
ALU = mybir.AluOpType
AX = mybir.AxisListType

ENGS = ["pe", "act", "dve", "pool", "sp"]
SAME_ENGINE_SYNC = True
NDMA_SEM = 24
DMAQ = ["sp", "pool"]

L_ALL = 4
T = 4096
D = 1024
DFF = 2752
NEG = -30000.0
EPS = 1e-6


class Buf:
    __slots__ = ("name", "w", "r")

    def __init__(self, name=""):
        self.name = name
        self.w = None
        self.r = {}


def bufs(n, name=""):
    return [Buf(name + str(i)) for i in range(n)]


class Prog:
    def __init__(self, nc, sems, dma_sems):
        self.nc = nc
        self.sems = sems
        self.dma_sems = dma_sems
        self.ops = {e: [] for e in ENGS}
        self.seen = {e: {} for e in ENGS}
        self.seen_d = {e: set() for e in ENGS}
        self.needed = {e: set() for e in ENGS}
        self.cnt = {e: 0 for e in ENGS}
        self.base = {e: 0 for e in ENGS}
        self.rankmap = {e: {} for e in ENGS}
        self.ndma = 0
        self.dma_slot_last = {}
        self.dma_q_count = {e: 0 for e in ENGS}
        self.dma_info = {}
        self.dma_slot_uses = {}
        self.outstanding = []
        self.nblock = 0
        self.ninstr = 0

    def _deps(self, reads, writes):
        deps = []
        for b in reads:
            if b.w is not None:
                deps.append(b.w)
        for b in writes:
            if b.w is not None:
                deps.append(b.w)
            deps.extend(b.r.values())
        return deps

    def _waits(self, eng, deps):
        waits = []
        seen = self.seen[eng]
        sd = self.seen_d[eng]
        for t in deps:
            if t[0] == "c":
                _, e2, idx = t
                if e2 == eng and (eng == "pe" or not SAME_ENGINE_SYNC):
                    continue
                if seen.get(e2, 0) >= idx:
                    continue
                seen[e2] = idx
                waits.append(t)
                self.needed[e2].add(idx)
            else:
                if t in sd:
                    continue
                sd.add(t)
                waits.append(t)
        return waits

    def _commit(self, tok, reads, writes):
        for b in writes:
            b.w = tok
            b.r = {}
        key = tok[1] if tok[0] == "c" else tok
        for b in reads:
            b.r[key] = tok

    def op(self, eng, fn, reads=(), writes=()):
        deps = self._deps(reads, writes)
        waits = self._waits(eng, deps)
        self.cnt[eng] += 1
        idx = self.cnt[eng]
        tok = ("c", eng, idx)
        self.ops[eng].append(["c", fn, waits, idx])
        self._commit(tok, reads, writes)
        return tok

    def dma(self, queue, fn, reads=(), writes=()):
        deps = self._deps(reads, writes)
        slot = self.dma_q_count[queue] % NDMA_SEM
        self.dma_q_count[queue] += 1
        prev = self.dma_slot_last.get((queue, slot))
        if prev is not None:
            deps.append(prev)
        waits = self._waits(queue, deps)
        self.ndma += 1
        tok = ("d", self.ndma)
        uses = self.dma_slot_uses.get((queue, slot), 0) + 1
        self.dma_slot_uses[(queue, slot)] = uses
        self.dma_info[tok] = (queue, slot, 16 * uses)
        self.dma_slot_last[(queue, slot)] = tok
        self.ops[queue].append(["d", fn, waits, tok])
        self._commit(tok, reads, writes)
        self.outstanding.append(tok)
        return tok

    def flush(self):
        w = self._waits("sp", list(self.outstanding))
        if w:
            self.ops["sp"].append(["w", None, w, None])
        rank = {}
        for e in ENGS:
            r = {}
            n = self.base[e]
            for i in sorted(self.needed[e]):
                n += 1
                r[i] = n
            rank[e] = r
        sems, dma_sems = self.sems, self.dma_sems

        def emit_engine(e, h):
            for kind, fn, waits, ident in self.ops[e]:
                for t in waits:
                    if t[0] == "c":
                        h.wait_ge(sems[t[1]], rank[t[1]][t[2]])
                    else:
                        q, slot, val = self.dma_info[t]
                        h.wait_ge(dma_sems[q][slot], val)
                if kind == "w":
                    continue
                ins = fn(h)
                self.ninstr += 1
                if kind == "c":
                    if ident in self.needed[e]:
                        ins.then_inc(sems[e], 1)
                else:
                    q, slot, val = self.dma_info[ident]
                    ins.then_inc(dma_sems[q][slot], 16)

        with self.nc.Block() as block:
            @block.sync
            def _(e):
                emit_engine("sp", e)

            @block.tensor
            def _(e):
                emit_engine("pe", e)

            @block.scalar
            def _(e):
                emit_engine("act", e)

            @block.vector
            def _(e):
                emit_engine("dve", e)

            @block.gpsimd
            def _(e):
                emit_engine("pool", e)
        for e in ENGS:
            self.base[e] += len(self.needed[e])
            self.needed[e] = set()
            self.ops[e] = []
        for e in ENGS:
            for e2 in ENGS:
                self.seen[e][e2] = self.cnt[e2]
            self.seen_d[e] = set()
        self.dma_slot_last = {}
        self.outstanding = []
        self._all_done = True
        self.nblock += 1


_orig_deps = Prog._deps


def _deps_epoch(self, reads, writes):
    deps = _orig_deps(self, reads, writes)
    out = []
    for t in deps:
        if t[0] == "d":
            if t[1] <= getattr(self, "_dma_done_upto", 0):
                continue
        out.append(t)
    return out


Prog._deps = _deps_epoch
_orig_flush = Prog.flush


def _flush2(self):
    _orig_flush(self)
    self._dma_done_upto = self.ndma


Prog.flush = _flush2


def host_consts():
    c = {}
    k = np.arange(128)[:, None, None]
    ktl = np.arange(4)[None, :, None]
    q = np.arange(512)[None, None, :]
    c["cm"] = np.where(q >= ktl * 128 + k, 0.0, NEG).astype(np.float32).reshape(128, 2048)
    qt = np.arange(32)[None, :, None]
    j = np.arange(16)[None, None, :]
    qblk = qt // 2
    ones = np.ones((128, 1, 1))
    c["pm"] = (np.where(j < qblk, 0.0, NEG) * ones).astype(np.float32).reshape(128, 512)
    c["ownfix"] = (np.where(j >= qblk, 0.0, -1e9) * ones).astype(np.float32).reshape(128, 512)
    c["alibase"] = (-256.0 * np.maximum(qblk - j, 0) * ones).astype(np.float32).reshape(128, 512)
    kk = np.arange(T)
    kst = np.zeros((8, 32, T), np.float32)
    for h in range(8):
        slope = 2.0 ** (-(h + 1))
        for jb in range(16):
            kst[h, jb, jb * 256:(jb + 1) * 256] = 1.0
        kst[h, 16, :] = -slope
        kst[h, 17, :] = slope * (kk % 256)
    c["kstat"] = kst
    qs = np.zeros((16, T), np.float32)
    qs[0] = kk % 256
    qs[1] = 1.0
    c["qstat"] = qs
    s = np.arange(128)[:, None]
    t = np.arange(128)[None, :]
    same = (s // 64) == (t // 64)
    c["ltri"] = np.where(same & (s <= t), -1.0 / 16, 0.0).astype(np.float32)
    c["lrev"] = np.where(same & (s > t), -1.0 / 16, 0.0).astype(np.float32)
    tri = np.where(same & (s <= t), 1.0, 0.0).astype(np.float32)
    c["tri01"] = np.tile(tri[:, None, :], (1, 4, 1)).reshape(128, 512)
    c["hm"] = (np.arange(128)[:, None] // 32 == np.arange(4)[None, :]).astype(np.float32)
    c["ident"] = np.eye(128, dtype=np.float32)
    return c


CONST_SHAPES = {"cm": [128, 2048], "pm": [128, 512], "ownfix": [128, 512], "alibase": [128, 512],
                "kstat": [8, 32, T], "qstat": [16, T], "ltri": [128, 128], "lrev": [128, 128],
                "tri01": [128, 512], "hm": [128, 4], "ident": [128, 128]}

PARAM_SHAPES = {
    "xT": [D, T], "memT": [D, 256],
    "w_in": [L_ALL, D, 3088], "w_out": [L_ALL, D, D], "xattn_wq": [L_ALL, D, 256],
    "xattn_wkv": [L_ALL, D, 512], "xattn_wo": [L_ALL, 256, D], "ffn_w_up": [L_ALL, D, 2 * DFF],
    "ffn_w_down": [L_ALL, DFF, D],
    "gains": [128, (L_ALL * 4 + 1) * 8], "scw": [128, L_ALL * 2 * 4], "fcw": [128, L_ALL * 44 * 4],
    "wg_aug": [32, L_ALL * 128], "gn": [64, L_ALL],
}


def host_params(inp):
    p = {}
    L = L_ALL
    g = np.zeros((128, L * 4 + 1, 8), np.float32)
    for l in range(L):
        for k, nm in enumerate(["norm_mix_g", "norm_xattn_g", "norm_mem_g", "norm_ffn_g"]):
            g[:, l * 4 + k, :] = inp[nm][l].reshape(8, 128).T
    g[:, L * 4, :] = inp["final_norm_g"].reshape(8, 128).T
    p["gains"] = g.reshape(128, -1)
    sc = np.zeros((128, L, 2, 4), np.float32)
    for l in range(L):
        for ch in range(2):
            sc[:, l, ch, 0:3] = inp["sc_conv_w"][l][:, ch * 128:(ch + 1) * 128].T
            sc[:, l, ch, 3] = inp["sc_conv_b"][l][ch * 128:(ch + 1) * 128]
    p["scw"] = sc.reshape(128, -1)
    fc = np.zeros((128, L, 44, 4), np.float32)
    for l in range(L):
        for half in range(2):
            for jc in range(22):
                lo = jc * 128
                n = min(128, DFF - lo)
                cols = slice(half * DFF + lo, half * DFF + lo + n)
                fc[:n, l, half * 22 + jc, 0:3] = inp["ffn_conv_w"][l][:, cols].T
                fc[:n, l, half * 22 + jc, 3] = inp["ffn_conv_b"][l][cols]
    p["fcw"] = fc.reshape(128, -1)
    wg = np.zeros((32, L, 128), np.float32)
    for l in range(L):
        wg[0:16, l, :] = inp["gla_w_gate"][l]
        wg[16, l, :] = inp["gla_b_gate"][l]
    p["wg_aug"] = wg.reshape(32, -1)
    p["gn"] = np.ascontiguousarray(inp["gla_norm_g"].T)
    return p


def build(n_layers=L_ALL, debug=False, max_phase=9):
    nc = bass.Bass("TRN2", target_bir_lowering=False)
    I = {}
    for nm, shp in list(PARAM_SHAPES.items()) + list(CONST_SHAPES.items()):
        I[nm] = nc.dram_tensor(nm, shp, F32, kind="ExternalInput").ap()
    outT = nc.dram_tensor("outT", [D, T], F32, kind="ExternalOutput").ap()

    def scratch(nm, shp, dt):
        return nc.dram_tensor(nm, shp, dt, kind="Internal").ap()
    xs = scratch("xs", [8, 128, T], F32)
    q_d = scratch("q_d", [8, 64, T], BF16)
    k_d = scratch("k_d", [8, 64, T], BF16)
    v_d = scratch("v_d", [32, 128, 512], BF16)
    yconv_d = scratch("yconv_d", [2, 128, T], BF16)
    ymoba_d = scratch("ymoba_d", [8, 64, T], BF16)
    ygla_d = scratch("ygla_d", [4, 64, T], BF16)
    qd_d = scratch("qd_d", [128, T], BF16)
    kd_d = scratch("kd_d", [128, T], BF16)
    kend_d = scratch("kend_d", [32, 128, 512], BF16)
    vg_d = scratch("vg_d", [32, 128, 256], BF16)
    sg_d = scratch("sg_d", [4, 64, T], BF16)
    dbg = {}
    if debug:
        dbg["yconv"] = nc.dram_tensor("dbg_yconv", [2, 128, T], BF16, kind="ExternalOutput").ap()
        dbg["ymoba"] = nc.dram_tensor("dbg_ymoba", [8, 64, T], BF16, kind="ExternalOutput").ap()
        dbg["ygla"] = nc.dram_tensor("dbg_ygla", [4, 64, T], BF16, kind="ExternalOutput").ap()
        dbg["x2"] = nc.dram_tensor("dbg_x2", [8, 128, T], F32, kind="ExternalOutput").ap()

    B_xs = bufs(8, "xs")
    B_q = [bufs(8, "q%d_" % h) for h in range(8)]
    B_k = [bufs(8, "k%d_" % h) for h in range(8)]
    B_v = bufs(32, "v")
    B_yconv = bufs(8, "yc")
    B_ymoba = [bufs(8, "ym%d_" % h) for h in range(8)]
    B_ygla = bufs(32, "yg")
    B_qd = bufs(8, "qd")
    B_kd = bufs(8, "kd")
    B_kend = bufs(32, "kend")
    B_vg = bufs(32, "vg")
    B_sg = bufs(8, "sg")

    with ExitStack() as top:
        sems = {e: top.enter_context(nc.semaphore("s_" + e)) for e in ENGS}
        dma_sems = {q: [top.enter_context(nc.semaphore("d_%s_%d" % (q, i))) for i in range(NDMA_SEM)] for q in DMAQ}
        P = Prog(nc, sems, dma_sems)

        uid = [0]

        def sbt(es, name, shape, dt):
            uid[0] += 1
            return es.enter_context(nc.sbuf_tensor("sb%d_%s" % (uid[0], name), shape, dt))

        gains = sbt(top, "gains", [128, (L_ALL * 4 + 1) * 8], F32)
        scw = sbt(top, "scw", [128, L_ALL * 2 * 4], F32)
        fcw = sbt(top, "fcw", [128, L_ALL * 44 * 4], F32)
        wg = sbt(top, "wg", [32, L_ALL * 128], F32)
        gn = sbt(top, "gn", [64, L_ALL], F32)
        hm = sbt(top, "hm", [128, 4], F32)
        ident = sbt(top, "ident", [128, 128], BF16)
        ones_bf = sbt(top, "ones_bf", [128, 128], BF16)
        ones_f = sbt(top, "ones_f", [128, 64], F32)
        dec = sbt(top, "dec", [128, 64], F32)
        B_dec = Buf("dec")
        B_const = Buf("const")
        banks = [top.enter_context(nc.psum_tensor("bank%d" % i, [128, 512], F32)) for i in range(8)]
        B_bank = bufs(8, "bank")

        class Rot:
            def __init__(self, ids):
                self.ids = ids
                self.i = 0

            def next(self):
                k = self.ids[self.i % len(self.ids)]
                self.i += 1
                return banks[k], B_bank[k]

        def MM(out, lhsT, rhs, start, stop, reads, writes, **kw):
            P.op("pe", lambda e: e.matmul(out, lhsT=lhsT, rhs=rhs, start=start, stop=stop, **kw), reads, writes)

        def ACT(out, in_, func, reads, writes, **kw):
            P.op("act", lambda e: e.activation(out=out, in_=in_, func=func, **kw), reads, writes)

        def TT(out, in0, in1, op, reads, writes, eng="dve"):
            P.op(eng, lambda e: e.tensor_tensor(out=out, in0=in0, in1=in1, op=op), reads, writes)

        def STT(out, in0, scalar, in1, op0, op1, reads, writes):
            P.op("dve", lambda e: e.scalar_tensor_tensor(out=out, in0=in0, scalar=scalar, in1=in1, op0=op0, op1=op1), reads, writes)

        def TS(out, in0, s1, s2, op0, op1, reads, writes, eng="dve"):
            if op1 is None:
                P.op(eng, lambda e: e.tensor_scalar(out=out, in0=in0, scalar1=s1, scalar2=None, op0=op0), reads, writes)
            else:
                P.op(eng, lambda e: e.tensor_scalar(out=out, in0=in0, scalar1=s1, scalar2=s2, op0=op0, op1=op1), reads, writes)

        def CP(out, in_, reads, writes, eng="dve"):
            P.op(eng, lambda e: e.tensor_copy(out=out, in_=in_), reads, writes)

        def MEMSET(ap, val, writes, eng="dve"):
            P.op(eng, lambda e: e.memset(ap, val), (), writes)

        def DMA(q, out, in_, reads, writes):
            P.dma(q, lambda e: e.dma_start(out=out, in_=in_), reads, writes)

        def gcol(l, kind, c):
            i = ((l * 4 + kind) * 8 + c) if l < L_ALL else (L_ALL * 4 * 8 + c)
            return gains[:, i:i + 1]

        DMA("sp", gains[:], I["gains"], (), [B_const])
        DMA("sp", scw[:], I["scw"], (), [B_const])
        DMA("sp", fcw[:], I["fcw"], (), [B_const])
        DMA("sp", wg[:], I["wg_aug"], (), [B_const])
        DMA("sp", gn[:], I["gn"], (), [B_const])
        DMA("sp", hm[:], I["hm"], (), [B_const])
        DMA("pool", ident[:], I["ident"], (), [B_const])
        MEMSET(ones_bf[:], 1.0, [B_const])
        MEMSET(ones_f[:], 1.0, [B_const])
        P.flush()

        def rmsnorm_tile(xt, Bx, ntok, l, kind, sq, Bsq, rstd, Brstd, hout, Bh, rot):
            ACT(sq[:, :, 0:ntok], xt[:, :, 0:ntok], AF.Square, [Bx], [Bsq])
            bk, Bb = rot.next()
            for c in range(8):
                MM(bk[:, 0:ntok], ones_bf[:], sq[:, c, 0:ntok], c == 0, c == 7, [Bsq, B_const], [Bb])
            ACT(rstd[:, 0:ntok], bk[:, 0:ntok], AF.Ln, [Bb], [Brstd], scale=1.0 / D, bias=EPS)
            ACT(rstd[:, 0:ntok], rstd[:, 0:ntok], AF.Exp, [Brstd], [Brstd], scale=-0.5)
            for c in range(8):
                STT(hout[:, c, 0:ntok], xt[:, c, 0:ntok], gcol(l, kind, c), rstd[:, 0:ntok], ALU.mult, ALU.mult,
                    [Bx, Brstd, B_const], [Bh[c]])

        def softmax_norm(po, Bpo, ydst, By, es_bufs, rot_bc):
            lnd, rden, bc_sb, Bn = es_bufs
            ACT(lnd[64:65, :], po[64:65, :], AF.Ln, [Bpo], [Bn[0]])
            ACT(rden[64:65, :], lnd[64:65, :], AF.Exp, [Bn[0]], [Bn[1]], scale=-1.0)
            bk, Bb = rot_bc.next()
            MM(bk[0:64, :], ones_f[64:65, 0:64], rden[64:65, :], True, True, [Bn[1], B_const], [Bb])
            ACT(bc_sb[0:64, :], bk[0:64, :], AF.Copy, [Bb], [Bn[2]])
            TT(ydst, po[0:64, :], bc_sb[0:64, :], ALU.mult, [Bpo, Bn[2]], By)

        for l in range(n_layers):
            xsrc = (lambda c, t0, n: I["xT"][c * 128:(c + 1) * 128, t0:t0 + n]) if l == 0 else None
            with ExitStack() as es:
                win = sbt(es, "win", [128, 8, 3088], BF16)
                B_win = Buf("win")
                xt2 = [sbt(es, "xt%d" % i, [128, 8, 512], F32) for i in range(2)]
                B_xt2 = bufs(2, "xt")
                sq = sbt(es, "sq", [128, 8, 512], BF16); B_sq = Buf("sq")
                rstd = sbt(es, "rstd", [128, 512], F32); B_rstd = Buf("rstd")
                hh2 = [sbt(es, "hh%d" % i, [128, 8, 512], BF16) for i in range(2)]; B_hh2 = [bufs(8, "hh%d_" % i) for i in range(2)]
                hh = hh2[0]; B_hh = B_hh2[0]
                tmpc = sbt(es, "tmpc", [128, 512], F32); B_tmpc = Buf("tmpc")
                ubuf = [sbt(es, "ubuf%d" % i, [128, 514], F32) for i in range(2)]; B_ubuf = bufs(2, "ubuf")
                c1 = sbt(es, "c1", [128, 512], F32); B_c1 = Buf("c1")
                ytile = sbt(es, "ytile", [128, 2, 512], BF16); B_ytile = bufs(2, "ytile")
                qst = sbt(es, "qst", [128, 4, 512], BF16); B_qst = bufs(4, "qst")
                kst = sbt(es, "kst", [128, 4, 512], BF16); B_kst = bufs(4, "kst")
                vst = sbt(es, "vst", [128, 4, 512], BF16); B_vst = bufs(4, "vst")
                vgst = sbt(es, "vgst", [128, 4, 256], BF16); B_vgst = bufs(4, "vgst")
                kendst = sbt(es, "kendst", [128, 4, 640], BF16); B_kendst = bufs(4, "kendst")
                sgst = sbt(es, "sgst", [128, 2, 512], BF16); B_sgst = bufs(2, "sgst")
                glr = sbt(es, "glr", [32, 512], F32); B_glr = Buf("glr")
                e1 = sbt(es, "e1", [128, 128], F32); B_e1 = Buf("e1")
                spl = sbt(es, "spl", [128, 128], F32); B_spl = Buf("spl")
                E1 = sbt(es, "E1", [128, 512], F32); B_E1 = bufs(4, "E1")
                E2 = sbt(es, "E2", [128, 512], F32); B_E2 = bufs(4, "E2")
                E3 = sbt(es, "E3", [128, 128], F32); B_E3 = Buf("E3")
                ltri = sbt(es, "ltri", [128, 128], F32)
                lrev = sbt(es, "lrev", [128, 128], F32)
                qdst = sbt(es, "qdst", [128, 512], BF16); B_qdst = Buf("qdst")
                kdst = sbt(es, "kdst", [128, 512], BF16); B_kdst = Buf("kdst")
                B_c = Buf("p1const")
                rot = Rot([0, 1, 2, 3, 4, 5])
                rot_hold = Rot([6, 7])

                win_src = I["w_in"][l].rearrange("(c p) n -> p c n", p=128)
                B_winh = bufs(2, "winh")
                for hi, (a, b) in enumerate([(0, 1544), (1544, 3088)]):
                    DMA("pool", win[:, :, a:b], win_src[:, :, a:b], (), [B_winh[hi]])

                def winb(c0, c1):
                    r = []
                    if c0 < 1544:
                        r.append(B_winh[0])
                    if c1 > 1544:
                        r.append(B_winh[1])
                    return r
                DMA("sp", ltri[:], I["ltri"], (), [B_c])
                DMA("sp", lrev[:], I["lrev"], (), [B_c])
                MEMSET(ubuf[0][:, 0:2], 0.0, [B_ubuf[0]])
                MEMSET(ubuf[1][:, 0:2], 0.0, [B_ubuf[1]])
                MEMSET(glr[:], 1.0, [B_glr])
                MEMSET(kendst[:], 0.0, B_kendst)
                scl = lambda ch, k: scw[:, (l * 2 + ch) * 4 + k:(l * 2 + ch) * 4 + k + 1]

                def proj(out, cols, rows_rhs, reads_extra, Bb, M=None):
                    for c in range(8):
                        MM(out, win[:, c, cols[0]:cols[1]], hh[:, c, :], c == 0, c == 7, winb(cols[0], cols[1]) + [B_hh[c]], [Bb])

                def load_norm(tt):
                    t0 = tt * 512
                    xt = xt2[tt % 2]; Bx = B_xt2[tt % 2]
                    if l == 0:
                        DMA("sp", xt[:], I["xT"].rearrange("(c p) t -> p c t", p=128)[:, :, t0:t0 + 512], (), [Bx])
                    else:
                        DMA("sp", xt[:], xs.rearrange("c p t -> p c t")[:, :, t0:t0 + 512], [B_xs[tt]], [Bx])
                    rmsnorm_tile(xt, Bx, 512, l, 0, sq, B_sq, rstd, B_rstd, hh2[tt % 2], B_hh2[tt % 2], rot)

                load_norm(0)
                for tt in range(8):
                    t0 = tt * 512
                    hh = hh2[tt % 2]; B_hh = B_hh2[tt % 2]
                    for ch in range(2):
                        pc, Bpc = rot.next()
                        proj(pc[:], (256 + 128 * ch, 384 + 128 * ch), None, None, Bpc)
                        ACT(tmpc[:], pc[:], AF.Copy, [Bpc], [B_tmpc])
                        ph, Bph = rot.next()
                        proj(ph[:], (512 + 128 * ch, 640 + 128 * ch), None, None, Bph)
                        TT(ubuf[ch][:, 2:514], ph[:], tmpc[:], ALU.mult, [Bph, B_tmpc], [B_ubuf[ch]])
                        ACT(c1[:], ubuf[ch][:, 2:514], AF.Identity, [B_ubuf[ch], B_const], [B_c1], scale=scl(ch, 2), bias=scl(ch, 3))
                        STT(c1[:], ubuf[ch][:, 1:513], scl(ch, 1), c1[:], ALU.mult, ALU.add, [B_ubuf[ch], B_c1, B_const], [B_c1])
                        STT(c1[:], ubuf[ch][:, 0:512], scl(ch, 0), c1[:], ALU.mult, ALU.add, [B_ubuf[ch], B_c1, B_const], [B_c1])
                        CP(ubuf[ch][:, 0:2], ubuf[ch][:, 512:514], [B_ubuf[ch]], [B_ubuf[ch]])
                        pb, Bpb = rot.next()
                        proj(pb[:], (128 * ch, 128 * ch + 128), None, None, Bpb)
                        TT(ytile[:, ch, :], pb[:], c1[:], ALU.mult, [Bpb, B_c1], [B_ytile[ch]])
                    DMA("pool", yconv_d.rearrange("c p t -> p c t")[:, :, t0:t0 + 512], ytile[:], B_ytile, [B_yconv[tt]])
                    for hp in range(4):
                        pq, Bpq = rot.next()
                        proj(pq[:], (768 + 128 * hp, 896 + 128 * hp), None, None, Bpq)
                        ACT(qst[:, hp, :], pq[:], AF.Identity, [Bpq], [B_qst[hp]], scale=0.125)
                        for two in range(2):
                            h = 2 * hp + two
                            DMA("pool", q_d[h][:, t0:t0 + 512], qst[64 * two:64 * two + 64, hp, :], [B_qst[hp]], [B_q[h][tt]])
                        pk, Bpk = rot.next()
                        proj(pk[:], (1280 + 128 * hp, 1408 + 128 * hp), None, None, Bpk)
                        CP(kst[:, hp, :], pk[:], [Bpk], [B_kst[hp]])
                        for two in range(2):
                            h = 2 * hp + two
                            DMA("pool", k_d[h][:, t0:t0 + 512], kst[64 * two:64 * two + 64, hp, :], [B_kst[hp]], [B_k[h][tt]])
                    if tt + 1 < 8:
                        load_norm(tt + 1)
                    for sub in range(4):
                        pv, Bpv = rot.next()
                        for c in range(8):
                            MM(pv[:], hh[:, c, sub * 128:(sub + 1) * 128], win[:, c, 1792:2304], c == 0, c == 7,
                               [B_winh[1], B_hh[c]], [Bpv])
                        if sub % 2 == 0:
                            ACT(vst[:, sub, :], pv[:], AF.Copy, [Bpv], [B_vst[sub]])
                        else:
                            CP(vst[:, sub, :], pv[:], [Bpv], [B_vst[sub]])
                        DMA("pool", v_d[tt * 4 + sub], vst[:, sub, :], [B_vst[sub]], [B_v[tt * 4 + sub]])
                    pgq, Bpgq = rot_hold.next()
                    proj(pgq[:], (2304, 2432), None, None, Bpgq)
                    pgk, Bpgk = rot_hold.next()
                    proj(pgk[:], (2432, 2560), None, None, Bpgk)
                    plr, Bplr = rot.next()
                    proj(plr[0:16, :], (3072, 3088), None, None, Bplr)
                    ACT(glr[0:16, :], plr[0:16, :], AF.Copy, [Bplr], [B_glr])
                    for sub in range(4):
                        kt = tt * 4 + sub
                        pkv, Bpkv = rot.next()
                        for c in range(8):
                            MM(pkv[:, 0:384], hh[:, c, sub * 128:(sub + 1) * 128], win[:, c, 2432:2816], c == 0, c == 7,
                               [B_winh[1], B_hh[c]], [Bpkv])
                        CP(vgst[:, sub, :], pkv[:, 128:384], [Bpkv], [B_vgst[sub]])
                        DMA("pool", vg_d[kt], vgst[:, sub, :], [B_vgst[sub]], [B_vg[kt]])
                        pla, Bpla = rot.next()
                        MM(pla[:, 0:128], glr[0:32, sub * 128:(sub + 1) * 128], wg[0:32, l * 128:(l + 1) * 128], True, True,
                           [B_glr, B_const], [Bpla])
                        ACT(e1[:], pla[:, 0:128], AF.Exp, [Bpla], [B_e1], scale=-1.0)
                        ACT(spl[:], e1[:], AF.Ln, [B_e1], [B_spl], bias=1.0)
                        pbt, Bpbt = rot.next()
                        MM(pbt[:, 0:128], spl[:], ltri[:], True, True, [B_spl, B_c], [Bpbt])
                        MM(pbt[:, 128:256], lrev[:], spl[:], True, True, [B_spl, B_c], [Bpbt])
                        ACT(E1[:, sub * 128:(sub + 1) * 128], pbt[:, 0:128], AF.Exp, [Bpbt], [B_E1[sub]])
                        ACT(E2[:, sub * 128:(sub + 1) * 128], pbt[:, 0:128], AF.Exp, [Bpbt], [B_E2[sub]], scale=-1.0)
                        ACT(E3[:], pbt[:, 128:256], AF.Exp, [Bpbt], [B_E3])
                        TT(kendst[:, sub, :].rearrange("p (h x) -> p h x", x=160)[:, :, 0:32],
                           pkv[:, 0:128].rearrange("p (h x) -> p h x", x=32),
                           E3[:].rearrange("p (h x) -> p h x", x=32), ALU.mult, [Bpkv, B_E3], [B_kendst[sub]])
                        DMA("pool", kend_d[kt], kendst[:, sub, 0:512], [B_kendst[sub]], [B_kend[kt]])
                        CP(dec[:, 2 * kt:2 * kt + 2], E1[:, sub * 128 + 63:sub * 128 + 128:64], [B_E1[sub]], [B_dec])
                    STT(qdst[:], pgq[:], 32.0 ** -0.5, E1[:], ALU.mult, ALU.mult, [Bpgq] + B_E1, [B_qdst])
                    DMA("pool", qd_d[:, t0:t0 + 512], qdst[:], [B_qdst], [B_qd[tt]])
                    TT(kdst[:], pgk[:], E2[:], ALU.mult, [Bpgk] + B_E2, [B_kdst])
                    DMA("pool", kd_d[:, t0:t0 + 512], kdst[:], [B_kdst], [B_kd[tt]])
                    for hp in range(2):
                        pgo, Bpgo = rot.next()
                        proj(pgo[:], (2816 + 128 * hp, 2944 + 128 * hp), None, None, Bpgo)
                        ACT(sgst[:, hp, :], pgo[:], AF.Silu, [Bpgo], [B_sgst[hp]])
                    DMA("pool", sg_d.rearrange("(hp two) p t -> (two p) hp t", two=2)[:, :, t0:t0 + 512], sgst[:], B_sgst, [B_sg[tt]])
                P.flush()

            if max_phase < 2:
                break
            with ExitStack() as es:
                QA = [sbt(es, "QA%d" % i, [96, T], BF16) for i in range(2)]
                KA = [sbt(es, "KA%d" % i, [96, T], BF16) for i in range(2)]
                VA = [sbt(es, "VA%d" % i, [128, 32, 65], BF16) for i in range(2)]
                B_QAm = bufs(2, "QAm"); B_KA = bufs(2, "KA"); B_VA = bufs(2, "VA")
                B_QAmask = [bufs(8, "QAmask%d_" % i) for i in range(2)]
                cm = sbt(es, "cm", [128, 4, 512], BF16)
                pmt = sbt(es, "pmt", [128, 32, 16], F32)
                ownfix = sbt(es, "ownfix", [128, 32, 16], F32)
                alibase = sbt(es, "alibase", [128, 32, 16], F32)
                B_c = Buf("p2const")
                km32 = sbt(es, "km32", [64, 16], F32); B_km32 = Buf("km32")
                kmb2 = [sbt(es, "kmb%d" % i, [64, 16], BF16) for i in range(2)]; B_kmb2 = bufs(2, "kmb")
                gm = sbt(es, "gm", [128, 4, 16], F32); B_gm = Buf("gm")
                top8 = sbt(es, "top8", [128, 4, 8], F32); B_top8 = Buf("top8")
                sel = sbt(es, "sel", [128, 4, 16], F32); B_sel = Buf("sel")
                stage = sbt(es, "stage", [128, 4, 128], BF16); B_stage = Buf("stage")
                pt = [sbt(es, "pt%d" % i, [128, 512], BF16) for i in range(8)]; B_pt = bufs(8, "pt")
                lnd = sbt(es, "lnd", [128, 512], F32)
                rden = sbt(es, "rden", [128, 512], F32)
                bc_sb = sbt(es, "bc_sb", [64, 512], F32)
                B_n = bufs(3, "nrm")
                yst = [sbt(es, "yst%d" % i, [64, 512], BF16) for i in range(2)]; B_yst = bufs(2, "yst")
                rot_s = Rot([0, 1, 2, 3, 6])
                rot_o = Rot([4, 5])
                rot_g = Rot([7])
                rot_t = Rot([7])

                DMA("pool", cm[:].rearrange("p a b -> p (a b)"), I["cm"], (), [B_c])
                DMA("sp", pmt[:].rearrange("p a b -> p (a b)"), I["pm"], (), [B_c])
                DMA("sp", ownfix[:].rearrange("p a b -> p (a b)"), I["ownfix"], (), [B_c])
                DMA("sp", alibase[:].rearrange("p a b -> p (a b)"), I["alibase"], (), [B_c])
                MEMSET(stage[:], 0.0, [B_stage])
                for i in range(2):
                    for hf in range(2):
                        DMA("pool", QA[i][80:96, hf * 2048:(hf + 1) * 2048], I["qstat"][:, hf * 2048:(hf + 1) * 2048], (), [B_QAm[i]])
                    MEMSET(VA[i][:, :, 64:65], 1.0, [B_VA[i]])
                ycnt = [0]

                def prologue(h):
                    bi = h % 2
                    DMA("sp", QA[bi][0:64, :], q_d[h], B_q[h], [B_QAm[bi]])
                    DMA("sp", KA[bi][0:64, :], k_d[h], B_k[h], [B_KA[bi]])
                    for hf in range(2):
                        DMA("pool", KA[bi][64:96, hf * 2048:(hf + 1) * 2048], I["kstat"][h][:, hf * 2048:(hf + 1) * 2048], (), [B_KA[bi]])
                    DMA("sp", VA[bi][:, :, 0:64], v_d.rearrange("k p (h e) -> p k h e", e=64)[:, :, h, :], B_v, [B_VA[bi]])

                def kmean(h):
                    bi = h % 2
                    P.op("dve", lambda e, bi=bi: e.tensor_reduce(out=km32[:], in_=KA[bi][0:64, :].rearrange("p (j k) -> p j k", k=256),
                                                                axis=AX.X, op=ALU.add), [B_KA[bi]], [B_km32])
                    ACT(kmb2[bi][:], km32[:], AF.Identity, [B_km32], [B_kmb2[bi]], scale=1.0 / 256)

                def gate_mm(h, g):
                    bi = h % 2
                    slope = 2.0 ** (-(h + 1))
                    pg, Bpg = rot_g.next()
                    for qt in range(4):
                        q0 = g * 512 + qt * 128
                        MM(pg[:, qt * 16:(qt + 1) * 16], QA[bi][0:64, q0:q0 + 128], kmb2[bi][:], True, True,
                           [B_QAm[bi], B_kmb2[bi]], [Bpg])
                    TT(gm[:], pg[:, 0:64].rearrange("p (a b) -> p a b", b=16), pmt[:, 4 * g:4 * g + 4, :], ALU.add,
                       [Bpg, B_c], [B_gm])
                    for qt in range(4):
                        P.op("dve", lambda e, qt=qt: e.max(out=top8[:, qt, :], in_=gm[:, qt, :]), [B_gm], [B_top8])
                    for qt in range(4):
                        TS(sel[:, qt, :], gm[:, qt, :], top8[:, qt, 2:3], None, ALU.is_ge, None, [B_gm, B_top8], [B_sel])
                    TS(sel[:], sel[:], -1.0, -NEG, ALU.add, ALU.mult, [B_sel], [B_sel])
                    TT(sel[:], sel[:], ownfix[:, 4 * g:4 * g + 4, :], ALU.max, [B_sel, B_c], [B_sel])
                    STT(stage[:, :, 64:80], alibase[:, 4 * g:4 * g + 4, :], slope, sel[:], ALU.mult, ALU.add,
                        [B_sel, B_c], [B_stage])

                def gate_T(h, g):
                    bi = h % 2
                    pT, BpT = rot_t.next()
                    for qt in range(4):
                        MM(pT[:, qt * 128:(qt + 1) * 128], stage[:, qt, :], ident[:], True, True, [B_stage, B_const], [BpT])
                    ACT(QA[bi][64:80, g * 512:(g + 1) * 512], pT[64:80, :], AF.Copy, [BpT], [B_QAmask[bi][g]])

                NPT = len(pt)
                LOOK = 4
                pend_norm = []

                def attention(h, g):
                    bi = h % 2
                    nkt = 4 * (g + 1)
                    po, Bpo = rot_o.next()
                    qs = QA[bi][0:96, g * 512:(g + 1) * 512]

                    def S_tile(kt):
                        pS, BpS = rot_s.next()
                        diag = kt >= 4 * g
                        MM(pS[:], KA[bi][0:96, kt * 128:(kt + 1) * 128], qs, True, not diag,
                           [B_KA[bi], B_QAm[bi], B_QAmask[bi][g]], [BpS])
                        if diag:
                            MM(pS[:], ident[:], cm[:, kt - 4 * g, :], False, True, [B_const, B_c], [BpS])
                        j = kt % NPT
                        ACT(pt[j][:], pS[:], AF.Exp, [BpS], [B_pt[j]])

                    def PV_tile(kt):
                        j = kt % NPT
                        MM(po[0:65, :], VA[bi][:, kt, 0:65], pt[j][:], kt == 0, kt == nkt - 1, [B_VA[bi], B_pt[j]], [Bpo])
                    for kt in range(min(LOOK, nkt)):
                        S_tile(kt)
                    for kt in range(nkt):
                        if kt + LOOK < nkt:
                            S_tile(kt + LOOK)
                        PV_tile(kt)
                        if kt == 3 and pend_norm:
                            pend_norm.pop(0)()
                    yi = ycnt[0] % 2
                    ycnt[0] += 1
                    ACT(lnd[64:65, :], po[64:65, :], AF.Ln, [Bpo], [B_n[0]])
                    ACT(rden[64:65, :], lnd[64:65, :], AF.Exp, [B_n[0]], [B_n[1]], scale=-1.0)

                    def rest(po=po, Bpo=Bpo, yi=yi, h=h, g=g):
                        bk, Bb = rot_t.next()
                        MM(bk[0:64, :], ones_f[64:65, 0:64], rden[64:65, :], True, True, [B_n[1], B_const], [Bb])
                        ACT(bc_sb[0:64, :], bk[0:64, :], AF.Copy, [Bb], [B_n[2]])
                        TT(yst[yi][:], po[0:64, :], bc_sb[0:64, :], ALU.mult, [Bpo, B_n[2]], [B_yst[yi]])
                        DMA("pool", ymoba_d[h][:, g * 512:(g + 1) * 512], yst[yi][:], [B_yst[yi]], [B_ymoba[h][g]])
                    pend_norm.append(rest)

                prologue(0)
                kmean(0)
                for g in range(8):
                    gate_mm(0, g)
                    gate_T(0, g)
                pending = []
                for h in range(8):
                    if h + 1 < 8:
                        prologue(h + 1)
                    for g in range(8):
                        if g == 5 and h + 1 < 8:
                            kmean(h + 1)
                            pending.extend((h + 1, gg) for gg in range(8))
                        task = pending.pop(0) if pending else None
                        if task:
                            gate_mm(*task)
                        attention(h, g)
                        if task:
                            gate_T(*task)
                assert not pending
                while pend_norm:
                    pend_norm.pop(0)()
                qdT = sbt(es, "qdT", [128, T], BF16); B_qdT = Buf("qdT")
                kdT = sbt(es, "kdT", [128, T], BF16); B_kdT = Buf("kdT")
                kend = sbt(es, "kend", [128, 32, 512], BF16); B_kendS = Buf("kendS")
                vg = sbt(es, "vg", [128, 32, 256], BF16); B_vgS = Buf("vgS")
                sg = sbt(es, "sg", [64, 4, T], BF16); B_sgS = Buf("sgS")
                Sall = sbt(es, "Sall", [128, 65, 64], F32); B_S = bufs(65, "S")
                Sbf = sbt(es, "Sbf", [128, 64, 64], BF16); B_Sbf = Buf("Sbf")
                tri01 = sbt(es, "tri01", [128, 512], BF16); B_c = Buf("p3const")
                qbd = [sbt(es, "qbd%d" % i, [128, 4, 128], BF16) for i in range(2)]; B_qbd = bufs(2, "qbd")
                Am = [sbt(es, "Am%d" % i, [128, 4, 128], BF16) for i in range(2)]; B_Am = bufs(2, "Am")
                sqg = sbt(es, "sqg", [64, 512], BF16); B_sqg = Buf("sqg")
                rs = sbt(es, "rs", [64, 512], F32); B_rs = Buf("rs")
                t1 = sbt(es, "t1", [64, 512], F32); B_t1 = Buf("t1")
                ygst = [sbt(es, "ygst%d" % i, [64, 4, 128], BF16) for i in range(2)]; B_ygst = bufs(2, "ygst")
                rot_u = Rot([0, 1])
                rot_a = Rot([2, 3])
                rot_o = Rot([4, 5])
                rot_s = Rot([6, 7])

                DMA("pool", tri01[:], I["tri01"], (), [B_c])
                DMA("sp", qdT[:], qd_d, B_qd, [B_qdT])
                DMA("sp", kdT[:], kd_d, B_kd, [B_kdT])
                for part in range(4):
                    DMA("sp", kend[:, part * 8:(part + 1) * 8, :], kend_d.rearrange("k p x -> p k x")[:, part * 8:(part + 1) * 8, :],
                        B_kend[part * 8:(part + 1) * 8], [B_kendS])
                    DMA("sp", vg[:, part * 8:(part + 1) * 8, :], vg_d.rearrange("k p x -> p k x")[:, part * 8:(part + 1) * 8, :],
                        B_vg[part * 8:(part + 1) * 8], [B_vgS])
                DMA("sp", sg[:], sg_d.rearrange("h p t -> p h t"), B_sg, [B_sgS])
                MEMSET(Sall[:, 0, :], 0.0, [B_S[0]])
                rot_ue = Rot([0, 1])
                rot_uo = Rot([6, 7])
                for grp in range(4):
                    pUe, BpUe = rot_ue.next()
                    pUo, BpUo = rot_uo.next()
                    for n in range(16):
                        N = grp * 16 + n
                        kt = N // 2
                        p0 = 64 * (N % 2)
                        pU, BpU = (pUe, BpUe) if N % 2 == 0 else (pUo, BpUo)
                        cs = slice((n // 2) * 64, (n // 2) * 64 + 64)
                        for h in range(4):
                            MM(pU[:, cs], kend[p0:p0 + 64, kt, h * 128:(h + 1) * 128],
                               vg[p0:p0 + 64, kt, h * 64:(h + 1) * 64], h == 0, h == 3, [B_kendS, B_vgS], [BpU])
                    for n in range(16):
                        N = grp * 16 + n
                        pU, BpU = (pUe, BpUe) if N % 2 == 0 else (pUo, BpUo)
                        cs = slice((n // 2) * 64, (n // 2) * 64 + 64)
                        STT(Sall[:, N + 1, :], Sall[:, N, :], dec[:, N:N + 1], pU[:, cs], ALU.mult, ALU.add,
                            [B_S[N], B_dec, BpU], [B_S[N + 1]])
                ACT(Sbf[:].rearrange("p a b -> p (a b)"), Sall[:, 0:64, :].rearrange("p a b -> p (a b)"), AF.Copy, B_S[0:64], [B_Sbf])
                for kt in range(32):
                    i2 = kt % 2
                    tok = slice(kt * 128, (kt + 1) * 128)
                    for h in range(4):
                        TS(qbd[i2][:, h, :], qdT[:, tok], hm[:, h:h + 1], None, ALU.mult, None, [B_qdT, B_const], [B_qbd[i2]])
                    pA, BpA = rot_a.next()
                    MM(pA[:], kdT[:, tok], qbd[i2][:].rearrange("p a b -> p (a b)"), True, True, [B_kdT, B_qbd[i2]], [BpA])
                    TT(Am[i2][:].rearrange("p a b -> p (a b)"), pA[:], tri01[:], ALU.mult, [BpA, B_c], [B_Am[i2]])
                    po, Bpo = rot_o.next()
                    pov = po[0:64, :].rearrange("p (h c) -> p h c", c=128)
                    for half in range(2):
                        for h in range(4):
                            MM(po[0:64, h * 128 + 64 * half:h * 128 + 64 * half + 64], Sbf[:, 2 * kt + half, :],
                               qbd[i2][:, h, 64 * half:64 * half + 64],
                               half == 0 and h == 0, False, [B_Sbf, B_qbd[i2]], [Bpo], skip_group_check=True)
                    for h in range(4):
                        MM(po[0:64, h * 128:(h + 1) * 128], vg[:, kt, h * 64:(h + 1) * 64], Am[i2][:, h, :], False, h == 3,
                           [B_vgS, B_Am[i2]], [Bpo], skip_group_check=True)
                    ACT(sqg[:], po[0:64, :], AF.Square, [Bpo], [B_sqg])
                    pss, Bpss = rot_s.next()
                    MM(pss[0:64, :], ones_bf[0:64, 0:64], sqg[:], True, True, [B_sqg, B_const], [Bpss])
                    ACT(rs[:], pss[0:64, :], AF.Ln, [Bpss], [B_rs], scale=1.0 / 64, bias=EPS)
                    ACT(rs[:], rs[:], AF.Exp, [B_rs], [B_rs], scale=-0.5)
                    STT(t1[:], po[0:64, :], gn[:, l:l + 1], rs[:], ALU.mult, ALU.mult, [Bpo, B_rs, B_const], [B_t1])
                    TT(ygst[i2][:], t1[:].rearrange("p (h c) -> p h c", c=128), sg[:, :, tok], ALU.mult, [B_t1, B_sgS], [B_ygst[i2]])
                    DMA("pool", ygla_d.rearrange("h p t -> p h t")[:, :, tok], ygst[i2][:], [B_ygst[i2]], [B_ygla[kt]])
                P.flush()

            if max_phase < 4:
                if debug:
                    DMA("sp", dbg["yconv"], yconv_d, B_yconv, [Buf()])
                    DMA("sp", dbg["ymoba"], ymoba_d, [b for hb in B_ymoba for b in hb], [Buf()])
                    DMA("sp", dbg["ygla"], ygla_d, B_ygla, [Buf()])
                    P.flush()
                break
            with ExitStack() as es:
                wo_c = sbt(es, "wo_c", [128, 2, D], BF16)
                wo_m = sbt(es, "wo_m", [128, 4, D], BF16)
                wo_g = sbt(es, "wo_g", [128, 2, D], BF16)
                wq = sbt(es, "wq", [128, 8, 256], BF16)
                wkv = sbt(es, "wkv", [128, 8, 512], BF16)
                wo_x = sbt(es, "wo_x", [128, 4, D], BF16)
                B_w = Buf("p4w")
                memt = sbt(es, "memt", [128, 8, 256], F32); B_memt = Buf("memt")
                memn = sbt(es, "memn", [128, 8, 256], BF16); B_memn = bufs(8, "memn")
                KmT = sbt(es, "KmT", [128, 4, 256], BF16); B_KmT = Buf("KmT")
                Vm = sbt(es, "Vm", [128, 2, 4, 65], BF16); B_Vm = Buf("Vm")
                xt2 = [sbt(es, "xt%d" % i, [128, 8, 512], F32) for i in range(2)]; B_xt2 = [bufs(8, "xt%d_" % i) for i in range(2)]
                B_xtall = bufs(2, "xtall")
                yc = sbt(es, "yc", [128, 2, 512], BF16); B_yc = Buf("yc")
                ym = sbt(es, "ym", [128, 4, 512], BF16); B_ym = Buf("ym")
                yg = sbt(es, "yg", [128, 2, 512], BF16); B_yg = Buf("yg")
                sq = sbt(es, "sq", [128, 8, 512], BF16); B_sq = Buf("sq")
                rstd = sbt(es, "rstd", [128, 512], F32); B_rstd = Buf("rstd")
                hh = sbt(es, "hh", [128, 8, 512], BF16); B_hh = bufs(8, "hh")
                qx4 = [sbt(es, "qx%d" % i, [128, 512], BF16) for i in range(4)]; B_qx4 = bufs(4, "qx")
                pt8 = [sbt(es, "pt%d" % i, [128, 512], BF16) for i in range(8)]; B_pt8 = bufs(8, "pt")
                ox = sbt(es, "ox", [128, 4, 512], BF16); B_ox = bufs(4, "ox")
                nset = [(sbt(es, "lnd%d" % i, [128, 512], F32), sbt(es, "rden%d" % i, [128, 512], F32),
                         sbt(es, "bc_sb%d" % i, [64, 512], F32), bufs(3, "nrm%d_" % i)) for i in range(2)]
                rot = Rot([0, 1, 2, 3, 4])
                rot_o = Rot([5, 6])
                rot_t = Rot([7])

                wsrc = I["w_out"][l]
                DMA("pool", wo_c[:], wsrc[0:256].rearrange("(c p) n -> p c n", p=128), (), [B_w])
                DMA("pool", wo_m[:], wsrc[256:768].rearrange("(h p) n -> p h n", p=128), (), [B_w])
                DMA("pool", wo_g[:], wsrc[768:1024].rearrange("(h p) n -> p h n", p=128), (), [B_w])
                DMA("pool", wq[:], I["xattn_wq"][l].rearrange("(c p) n -> p c n", p=128), (), [B_w])
                DMA("pool", wkv[:], I["xattn_wkv"][l].rearrange("(c p) n -> p c n", p=128), (), [B_w])
                MEMSET(wo_x[64:128, :, :], 0.0, [B_w])
                MEMSET(KmT[64:128, :, :], 0.0, [B_KmT])
                MEMSET(ox[64:128, :, :], 0.0, B_ox)
                for i in range(4):
                    MEMSET(qx4[i][64:128, :], 0.0, [B_qx4[i]])
                DMA("pool", wo_x[0:64, :, :], I["xattn_wo"][l].rearrange("(h p) n -> p h n", p=64), (), [B_w])
                DMA("sp", memt[:], I["memT"].rearrange("(c p) t -> p c t", p=128), (), [B_memt])
                MEMSET(Vm[:, :, :, 64:65], 1.0, [B_Vm])
                rmsnorm_tile(memt, B_memt, 256, l, 2, sq, B_sq, rstd, B_rstd, memn, B_memn, rot)
                for h in range(4):
                    pk, Bpk = rot.next()
                    for c in range(8):
                        MM(pk[0:64, 0:256], wkv[:, c, 64 * h:64 * h + 64], memn[:, c, :], c == 0, c == 7, [B_w, B_memn[c]], [Bpk])
                    CP(KmT[0:64, h, :], pk[0:64, 0:256], [Bpk], [B_KmT])
                for mt in range(2):
                    pv, Bpv = rot.next()
                    for c in range(8):
                        MM(pv[:, 0:256], memn[:, c, mt * 128:(mt + 1) * 128], wkv[:, c, 256:512], c == 0, c == 7, [B_w, B_memn[c]], [Bpv])
                    CP(Vm[:, mt, :, 0:64], pv[:, 0:256].rearrange("p (h e) -> p h e", e=64), [Bpv], [B_Vm])

                def Wst(tt):
                    t0 = tt * 512
                    xi = tt % 2
                    xt = xt2[xi]; Bx = B_xt2[xi]
                    if l == 0:
                        DMA("sp", xt[:], I["xT"].rearrange("(c p) t -> p c t", p=128)[:, :, t0:t0 + 512], (), Bx)
                    else:
                        DMA("sp", xt[:], xs.rearrange("c p t -> p c t")[:, :, t0:t0 + 512], [B_xs[tt]], Bx)
                    DMA("sp", yc[:], yconv_d.rearrange("c p t -> p c t")[:, :, t0:t0 + 512], [B_yconv[tt]], [B_yc])
                    DMA("sp", ym[:], ymoba_d.rearrange("(hp two) p t -> (two p) hp t", two=2)[:, :, t0:t0 + 512], [B_ymoba[h][tt] for h in range(8)], [B_ym])
                    DMA("sp", yg[:], ygla_d.rearrange("(hp two) p t -> (two p) hp t", two=2)[:, :, t0:t0 + 512], B_ygla[tt * 4:tt * 4 + 4], [B_yg])
                    for oc in range(8):
                        cs = slice(oc * 128, (oc + 1) * 128)
                        pw, Bpw = rot.next()
                        for c in range(2):
                            MM(pw[:], wo_c[:, c, cs], yc[:, c, :], c == 0, False, [B_w, B_yc], [Bpw])
                        for h in range(4):
                            MM(pw[:], wo_m[:, h, cs], ym[:, h, :], False, False, [B_w, B_ym], [Bpw])
                        for h in range(2):
                            MM(pw[:], wo_g[:, h, cs], yg[:, h, :], False, h == 1, [B_w, B_yg], [Bpw])
                        TT(xt[:, oc, :], pw[:], xt[:, oc, :], ALU.add, [Bpw, Bx[oc]], [Bx[oc]])

                def Nst(tt):
                    xi = tt % 2
                    xt = xt2[xi]; Bx = B_xt2[xi]
                    ACT(sq[:], xt[:], AF.Square, Bx, [B_sq])
                    bk, Bb = rot.next()
                    for c in range(8):
                        MM(bk[:], ones_bf[:], sq[:, c, :], c == 0, c == 7, [B_sq, B_const], [Bb])
                    ACT(rstd[:], bk[:], AF.Ln, [Bb], [B_rstd], scale=1.0 / D, bias=EPS)
                    ACT(rstd[:], rstd[:], AF.Exp, [B_rstd], [B_rstd], scale=-0.5)
                    for c in range(8):
                        STT(hh[:, c, :], xt[:, c, :], gcol(l, 1, c), rstd[:], ALU.mult, ALU.mult, [Bx[c], B_rstd, B_const], [B_hh[c]])

                def Xst(tt):
                    for h in range(4):
                        pq, Bpq = rot.next()
                        for c in range(8):
                            MM(pq[0:64, :], wq[:, c, 64 * h:64 * h + 64], hh[:, c, :], c == 0, c == 7, [B_w, B_hh[c]], [Bpq])
                        ACT(qx4[h][0:64, :], pq[0:64, :], AF.Identity, [Bpq], [B_qx4[h]], scale=0.125)
                    for h in range(4):
                        for mt in range(2):
                            pS, BpS = rot.next()
                            MM(pS[:], KmT[:, h, mt * 128:(mt + 1) * 128], qx4[h][:], True, True, [B_KmT, B_qx4[h]], [BpS])
                            ACT(pt8[h * 2 + mt][:], pS[:], AF.Exp, [BpS], [B_pt8[h * 2 + mt]])
                    for h in range(4):
                        po, Bpo = rot_o.next()
                        for mt in range(2):
                            MM(po[0:65, :], Vm[:, mt, h, 0:65], pt8[h * 2 + mt][:], mt == 0, mt == 1, [B_Vm, B_pt8[h * 2 + mt]], [Bpo])
                        softmax_norm(po, Bpo, ox[0:64, h, :], [B_ox[h]], nset[h % 2], rot_t)

                def Ost(tt):
                    t0 = tt * 512
                    xi = tt % 2
                    xt = xt2[xi]; Bx = B_xt2[xi]
                    for oc in range(8):
                        cs = slice(oc * 128, (oc + 1) * 128)
                        pw, Bpw = rot.next()
                        for h in range(4):
                            MM(pw[:], wo_x[:, h, cs], ox[:, h, :], h == 0, h == 3, [B_w, B_ox[h]], [Bpw])
                        TT(xt[:, oc, :], pw[:], xt[:, oc, :], ALU.add, [Bpw, Bx[oc]], [Bx[oc]])
                    DMA("pool", xs.rearrange("c p t -> p c t")[:, :, t0:t0 + 512], xt[:], Bx, [B_xs[tt]])

                Wst(0)
                for tt in range(8):
                    Nst(tt)
                    if tt + 1 < 8:
                        Wst(tt + 1)
                    Xst(tt)
                    Ost(tt)
                P.flush()

            if debug and l == 0:
                DMA("sp", dbg["yconv"], yconv_d, B_yconv, [Buf()])
                DMA("sp", dbg["ymoba"], ymoba_d, [b for hb in B_ymoba for b in hb], [Buf()])
                DMA("sp", dbg["ygla"], ygla_d, B_ygla, [Buf()])
                DMA("sp", dbg["x2"], xs, B_xs, [Buf()])
                P.flush()

            if max_phase < 6:
                break
            with ExitStack() as es:
                NTK = 256
                NT6 = T // NTK
                wup = sbt(es, "wup", [128, 8, 2 * DFF], BF16)
                wdn = sbt(es, "wdn", [128, 22, D], BF16)
                B_wup = [bufs(8, "wup%d_" % hf) for hf in range(2)]
                B_wdn = bufs(4, "wdn")
                xt2 = [sbt(es, "xt%d" % i, [128, 8, NTK], F32) for i in range(2)]; B_xt2 = [bufs(8, "xt%d_" % i) for i in range(2)]
                sq = sbt(es, "sq", [128, 8, NTK], BF16); B_sq = Buf("sq")
                rstd = sbt(es, "rstd", [128, NTK], F32); B_rstd = Buf("rstd")
                hh2 = [sbt(es, "hh%d" % i, [128, 8, NTK], BF16) for i in range(2)]; B_hh2 = [bufs(8, "hh%d_" % i) for i in range(2)]
                NB = 3
                ab = [sbt(es, "ab%d" % i, [128, 2, NTK + 2], F32) for i in range(NB)]; B_ab = bufs(NB, "ab")
                cc = [sbt(es, "cc%d" % i, [128, 2, NTK], F32) for i in range(NB)]; B_cc = [bufs(2, "cc%d_" % i) for i in range(NB)]
                sl = [sbt(es, "sl%d" % i, [128, NTK], F32) for i in range(NB)]; B_sl = bufs(NB, "sl")
                hst = sbt(es, "hst", [128, 22, 2, 2], F32); B_hst = bufs(22, "hst")
                hact = sbt(es, "hact", [128, 22, NTK], BF16); B_hact = bufs(22, "hact")
                rot = Rot([0, 1, 2, 3, 4, 5, 6, 7])

                up_src = I["ffn_w_up"][l].rearrange("(c p) n -> p c n", p=128)
                for grp4 in range(4):
                    for hf in range(2):
                        a = hf * DFF + grp4 * 688
                        DMA("pool", wup[:, :, a:a + 688], up_src[:, :, a:a + 688], (), [B_wup[hf][2 * grp4], B_wup[hf][2 * grp4 + 1]])
                dn_src = I["ffn_w_down"][l]
                for gi, jg in enumerate(range(0, 21, 7)):
                    DMA("pool", wdn[:, jg:jg + 7, :], dn_src[jg * 128:(jg + 7) * 128].rearrange("(j p) n -> p j n", p=128), (), [B_wdn[gi]])
                DMA("pool", wdn[0:64, 21, :], dn_src[2688:2752], (), [B_wdn[3]])
                MEMSET(hst[:].rearrange("p a b c -> p (a b c)"), 0.0, B_hst)
                fcl = lambda idx, k: fcw[:, (l * 44 + idx) * 4 + k:(l * 44 + idx) * 4 + k + 1]
                last = (l == n_layers - 1)

                def wup_bufs(hf, j, rows):
                    g0 = (j * 128) // 344
                    g1 = (j * 128 + rows - 1) // 344
                    return [B_wup[hf][g] for g in range(g0, g1 + 1)]

                def load_x(tt):
                    xi = tt % 2
                    DMA("sp", xt2[xi][:], xs.rearrange("c p t -> p c t")[:, :, tt * NTK:(tt + 1) * NTK], [B_xs[tt * NTK // 512]], B_xt2[xi])

                def norm_sq(tt):
                    ACT(sq[:], xt2[tt % 2][:], AF.Square, B_xt2[tt % 2], [B_sq])

                def norm_pe(tt, kind_l, kind):
                    bk, Bb = rot.next()
                    for c in range(8):
                        MM(bk[:, 0:NTK], ones_bf[:], sq[:, c, :], c == 0, c == 7, [B_sq, B_const], [Bb])
                    ACT(rstd[:], bk[:, 0:NTK], AF.Ln, [Bb], [B_rstd], scale=1.0 / D, bias=EPS)
                    ACT(rstd[:], rstd[:], AF.Exp, [B_rstd], [B_rstd], scale=-0.5)

                def norm_h(tt, c):
                    xi = tt % 2
                    STT(hh2[xi][:, c, :], xt2[xi][:, c, :], gcol(l, 3, c), rstd[:], ALU.mult, ALU.mult,
                        [B_xt2[xi][c], B_rstd, B_const], [B_hh2[xi][c]])

                load_x(0)
                norm_sq(0)
                norm_pe(0, l, 3)
                for c in range(8):
                    norm_h(0, c)
                for tt in range(NT6):
                    t0 = tt * NTK
                    xi = tt % 2
                    xt = xt2[xi]; Bx = B_xt2[xi]
                    hh = hh2[xi]; B_hh = B_hh2[xi]
                    pas = {}

                    def stageA(j):
                        rows = 128 if j < 21 else 64
                        i3 = j % NB
                        pa, Bpa = rot.next()
                        pas[j] = (pa, Bpa)
                        for half in range(2):
                            c0 = half * DFF + j * 128
                            wb = wup_bufs(half, j, rows)
                            for c in range(8):
                                MM(pa[0:rows, half * NTK:(half + 1) * NTK], wup[:, c, c0:c0 + rows], hh[:, c, :], c == 0, c == 7,
                                   wb + [B_hh[c]], [Bpa])
                        abt = ab[i3]
                        ACT(abt[0:rows, :, 2:NTK + 2], pa[0:rows, :].rearrange("p (a b) -> p a b", b=NTK), AF.Copy, [Bpa], [B_ab[i3]])
                        CP(abt[0:rows, :, 0:2], hst[0:rows, j, :, :], [B_hst[j]], [B_ab[i3]])

                    def stageB(j):
                        rows = 128 if j < 21 else 64
                        i3 = j % NB
                        abt = ab[i3]; cct = cc[i3]
                        for half in range(2):
                            idx = half * 22 + j
                            ACT(cct[0:rows, half, :], abt[0:rows, half, 2:NTK + 2], AF.Identity, [B_ab[i3], B_const], [B_cc[i3][half]],
                                scale=fcl(idx, 2)[0:rows], bias=fcl(idx, 3)[0:rows])
                        for k in (1, 0):
                            for half in range(2):
                                idx = half * 22 + j
                                STT(cct[0:rows, half, :], abt[0:rows, half, k:NTK + k], fcl(idx, k)[0:rows], cct[0:rows, half, :], ALU.mult, ALU.add,
                                    [B_ab[i3], B_cc[i3][half], B_const], [B_cc[i3][half]])
                        CP(hst[0:rows, j, :, :], abt[0:rows, :, NTK:NTK + 2], [B_ab[i3]], [B_hst[j]])

                    def stageC(j):
                        rows = 128 if j < 21 else 64
                        i3 = j % NB
                        cct = cc[i3]
                        ACT(sl[i3][0:rows, :], cct[0:rows, 0, :], AF.Silu, [B_cc[i3][0]], [B_sl[i3]])
                        TT(hact[0:rows, j, :], sl[i3][0:rows, :], cct[0:rows, 1, :], ALU.mult, [B_sl[i3], B_cc[i3][1]], [B_hact[j]])

                    for step in range(22 + 2):
                        if step < 22:
                            stageA(step)
                        if 0 <= step - 1 < 22:
                            stageB(step - 1)
                        if 0 <= step - 2 < 22:
                            stageC(step - 2)
                        if tt + 1 < NT6:
                            if step == 4:
                                load_x(tt + 1)
                            if step == 8:
                                norm_sq(tt + 1)
                            if step == 11:
                                norm_pe(tt + 1, l, 3)
                            if 13 <= step < 21:
                                norm_h(tt + 1, step - 13)
                    for oc in range(8):
                        cs = slice(oc * 128, (oc + 1) * 128)
                        pd, Bpd = rot.next()
                        for j in range(22):
                            rows = 128 if j < 21 else 64
                            MM(pd[:, 0:NTK], wdn[0:rows, j, cs], hact[0:rows, j, :], j == 0, j == 21, [B_wdn[min(j // 7, 3)], B_hact[j]], [Bpd])
                        TT(xt[:, oc, :], pd[:, 0:NTK], xt[:, oc, :], ALU.add, [Bpd, Bx[oc]], [Bx[oc]])
                    if not last:
                        DMA("pool", xs.rearrange("c p t -> p c t")[:, :, t0:t0 + NTK], xt[:], Bx, [B_xs[t0 // 512]])
                    else:
                        ACT(hact[:, 0:8, :], xt[:], AF.Square, Bx, B_hact[0:8])
                        bk, Bb = rot.next()
                        for c in range(8):
                            MM(bk[:, 0:NTK], ones_bf[:], hact[:, c, :], c == 0, c == 7, B_hact[0:8] + [B_const], [Bb])
                        ACT(sl[0][:], bk[:, 0:NTK], AF.Ln, [Bb], [B_sl[0]], scale=1.0 / D, bias=EPS)
                        ACT(sl[0][:], sl[0][:], AF.Exp, [B_sl[0]], [B_sl[0]], scale=-0.5)
                        for c in range(8):
                            STT(xt[:, c, :], xt[:, c, :], gcol(L_ALL, 0, c), sl[0][:], ALU.mult, ALU.mult, [Bx[c], B_sl[0], B_const], [Bx[c]])
                        DMA("pool", outT.rearrange("(c p) t -> p c t", p=128)[:, :, t0:t0 + NTK], xt[:], Bx, [Buf()])
                P.flush()
        build.ninstr = P.ninstr
    return nc


_CACHE = {}


def make_in_maps(inputs):
    consts = host_consts()
    params = host_params(inputs)
    shared = {}
    for nm in ["w_in", "w_out", "xattn_wq", "xattn_wkv", "xattn_wo", "ffn_w_up", "ffn_w_down"]:
        shared[nm] = np.ascontiguousarray(inputs[nm], dtype=np.float32)
    shared.update(params)
    shared.update(consts)
    in_maps = []
    for core in range(8):
        b = core % 4
        m = dict(shared)
        m["xT"] = np.ascontiguousarray(inputs["x"][b].T)
        m["memT"] = np.ascontiguousarray(inputs["mem"][b].T)
        in_maps.append(m)
    return in_maps


def kernel(**inputs):
    inputs = {k: np.asarray(v) for k, v in inputs.items()}
    if "nc" not in _CACHE:
        _CACHE["nc"] = build()
    nc = _CACHE["nc"]
    in_maps = make_in_maps(inputs)
    res = run_bass_kernel_spmd(nc, in_maps, core_ids=list(range(8)))
    out = np.stack([np.ascontiguousarray(res.results[b]["outT"].T) for b in range(4)], axis=0)
    return out.astype(np.float32)
```

```python
import numpy as np
from contextlib import ExitStack
import concourse.bass as bass
import concourse.mybir as mybir
from concourse.bass_utils import run_bass_kernel_spmd

F32 = mybir.dt.float32
BF16 = mybir.dt.bfloat16
AF = mybir.ActivationFunctionType
ALU = mybir.AluOpType
AX = mybir.AxisListType

ENGS = ["pe", "act", "dve", "pool", "sp"]
SAME_ENGINE_SYNC = True
NDMA_SEM = 24
DMAQ = ["sp", "pool"]

L_ALL = 4
T = 4096
D = 1024
DFF = 2752
NEG = -30000.0
EPS = 1e-6


class Buf:
    __slots__ = ("name", "w", "r")

    def __init__(self, name=""):
        self.name = name
        self.w = None
        self.r = {}


def bufs(n, name=""):
    return [Buf(name + str(i)) for i in range(n)]


class Prog:
    def __init__(self, nc, sems, dma_sems):
        self.nc = nc
        self.sems = sems
        self.dma_sems = dma_sems
        self.ops = {e: [] for e in ENGS}
        self.seen = {e: {} for e in ENGS}
        self.seen_d = {e: set() for e in ENGS}
        self.needed = {e: set() for e in ENGS}
        self.cnt = {e: 0 for e in ENGS}
        self.base = {e: 0 for e in ENGS}
        self.rankmap = {e: {} for e in ENGS}
        self.ndma = 0
        self.dma_slot_last = {}
        self.dma_q_count = {e: 0 for e in ENGS}
        self.dma_info = {}
        self.dma_slot_uses = {}
        self.outstanding = []
        self.nblock = 0
        self.ninstr = 0

    def _deps(self, reads, writes):
        deps = []
        for b in reads:
            if b.w is not None:
                deps.append(b.w)
        for b in writes:
            if b.w is not None:
                deps.append(b.w)
            deps.extend(b.r.values())
        return deps

    def _waits(self, eng, deps):
        waits = []
        seen = self.seen[eng]
        sd = self.seen_d[eng]
        for t in deps:
            if t[0] == "c":
                _, e2, idx = t
                if e2 == eng and (eng == "pe" or not SAME_ENGINE_SYNC):
                    continue
                if seen.get(e2, 0) >= idx:
                    continue
                seen[e2] = idx
                waits.append(t)
                self.needed[e2].add(idx)
            else:
                if t in sd:
                    continue
                sd.add(t)
                waits.append(t)
        return waits

    def _commit(self, tok, reads, writes):
        for b in writes:
            b.w = tok
            b.r = {}
        key = tok[1] if tok[0] == "c" else tok
        for b in reads:
            b.r[key] = tok

    def op(self, eng, fn, reads=(), writes=()):
        deps = self._deps(reads, writes)
        waits = self._waits(eng, deps)
        self.cnt[eng] += 1
        idx = self.cnt[eng]
        tok = ("c", eng, idx)
        self.ops[eng].append(["c", fn, waits, idx])
        self._commit(tok, reads, writes)
        return tok

    def dma(self, queue, fn, reads=(), writes=()):
        deps = self._deps(reads, writes)
        slot = self.dma_q_count[queue] % NDMA_SEM
        self.dma_q_count[queue] += 1
        prev = self.dma_slot_last.get((queue, slot))
        if prev is not None:
            deps.append(prev)
        waits = self._waits(queue, deps)
        self.ndma += 1
        tok = ("d", self.ndma)
        uses = self.dma_slot_uses.get((queue, slot), 0) + 1
        self.dma_slot_uses[(queue, slot)] = uses
        self.dma_info[tok] = (queue, slot, 16 * uses)
        self.dma_slot_last[(queue, slot)] = tok
        self.ops[queue].append(["d", fn, waits, tok])
        self._commit(tok, reads, writes)
        self.outstanding.append(tok)
        return tok

    def flush(self):
        w = self._waits("sp", list(self.outstanding))
        if w:
            self.ops["sp"].append(["w", None, w, None])
        rank = {}
        for e in ENGS:
            r = {}
            n = self.base[e]
            for i in sorted(self.needed[e]):
                n += 1
                r[i] = n
            rank[e] = r
        sems, dma_sems = self.sems, self.dma_sems

        def emit_engine(e, h):
            for kind, fn, waits, ident in self.ops[e]:
                for t in waits:
                    if t[0] == "c":
                        h.wait_ge(sems[t[1]], rank[t[1]][t[2]])
                    else:
                        q, slot, val = self.dma_info[t]
                        h.wait_ge(dma_sems[q][slot], val)
                if kind == "w":
                    continue
                ins = fn(h)
                self.ninstr += 1
                if kind == "c":
                    if ident in self.needed[e]:
                        ins.then_inc(sems[e], 1)
                else:
                    q, slot, val = self.dma_info[ident]
                    ins.then_inc(dma_sems[q][slot], 16)

        with self.nc.Block() as block:
            @block.sync
            def _(e):
                emit_engine("sp", e)

            @block.tensor
            def _(e):
                emit_engine("pe", e)

            @block.scalar
            def _(e):
                emit_engine("act", e)

            @block.vector
            def _(e):
                emit_engine("dve", e)

            @block.gpsimd
            def _(e):
                emit_engine("pool", e)
        for e in ENGS:
            self.base[e] += len(self.needed[e])
            self.needed[e] = set()
            self.ops[e] = []
        for e in ENGS:
            for e2 in ENGS:
                self.seen[e][e2] = self.cnt[e2]
            self.seen_d[e] = set()
        self.dma_slot_last = {}
        self.outstanding = []
        self._all_done = True
        self.nblock += 1


_orig_deps = Prog._deps


def _deps_epoch(self, reads, writes):
    deps = _orig_deps(self, reads, writes)
    out = []
    for t in deps:
        if t[0] == "d":
            if t[1] <= getattr(self, "_dma_done_upto", 0):
                continue
        out.append(t)
    return out


Prog._deps = _deps_epoch
_orig_flush = Prog.flush


def _flush2(self):
    _orig_flush(self)
    self._dma_done_upto = self.ndma


Prog.flush = _flush2


def host_consts():
    c = {}
    k = np.arange(128)[:, None, None]
    ktl = np.arange(4)[None, :, None]
    q = np.arange(512)[None, None, :]
    c["cm"] = np.where(q >= ktl * 128 + k, 0.0, NEG).astype(np.float32).reshape(128, 2048)
    qt = np.arange(32)[None, :, None]
    j = np.arange(16)[None, None, :]
    qblk = qt // 2
    ones = np.ones((128, 1, 1))
    c["pm"] = (np.where(j < qblk, 0.0, NEG) * ones).astype(np.float32).reshape(128, 512)
    c["ownfix"] = (np.where(j >= qblk, 0.0, -1e9) * ones).astype(np.float32).reshape(128, 512)
    c["alibase"] = (-256.0 * np.maximum(qblk - j, 0) * ones).astype(np.float32).reshape(128, 512)
    kk = np.arange(T)
    kst = np.zeros((8, 32, T), np.float32)
    for h in range(8):
        slope = 2.0 ** (-(h + 1))
        for jb in range(16):
            kst[h, jb, jb * 256:(jb + 1) * 256] = 1.0
        kst[h, 16, :] = -slope
        kst[h, 17, :] = slope * (kk % 256)
    c["kstat"] = kst
    qs = np.zeros((16, T), np.float32)
    qs[0] = kk % 256
    qs[1] = 1.0
    c["qstat"] = qs
    s = np.arange(128)[:, None]
    t = np.arange(128)[None, :]
    same = (s // 64) == (t // 64)
    c["ltri"] = np.where(same & (s <= t), -1.0 / 16, 0.0).astype(np.float32)
    c["lrev"] = np.where(same & (s > t), -1.0 / 16, 0.0).astype(np.float32)
    tri = np.where(same & (s <= t), 1.0, 0.0).astype(np.float32)
    c["tri01"] = np.tile(tri[:, None, :], (1, 4, 1)).reshape(128, 512)
    c["hm"] = (np.arange(128)[:, None] // 32 == np.arange(4)[None, :]).astype(np.float32)
    c["ident"] = np.eye(128, dtype=np.float32)
    return c


CONST_SHAPES = {"cm": [128, 2048], "pm": [128, 512], "ownfix": [128, 512], "alibase": [128, 512],
                "kstat": [8, 32, T], "qstat": [16, T], "ltri": [128, 128], "lrev": [128, 128],
                "tri01": [128, 512], "hm": [128, 4], "ident": [128, 128]}

PARAM_SHAPES = {
    "xT": [D, T], "memT": [D, 256],
    "w_in": [L_ALL, D, 3088], "w_out": [L_ALL, D, D], "xattn_wq": [L_ALL, D, 256],
    "xattn_wkv": [L_ALL, D, 512], "xattn_wo": [L_ALL, 256, D], "ffn_w_up": [L_ALL, D, 2 * DFF],
    "ffn_w_down": [L_ALL, DFF, D],
    "gains": [128, (L_ALL * 4 + 1) * 8], "scw": [128, L_ALL * 2 * 4], "fcw": [128, L_ALL * 44 * 4],
    "wg_aug": [32, L_ALL * 128], "gn": [64, L_ALL],
}


def host_params(inp):
    p = {}
    L = L_ALL
    g = np.zeros((128, L * 4 + 1, 8), np.float32)
    for l in range(L):
        for k, nm in enumerate(["norm_mix_g", "norm_xattn_g", "norm_mem_g", "norm_ffn_g"]):
            g[:, l * 4 + k, :] = inp[nm][l].reshape(8, 128).T
    g[:, L * 4, :] = inp["final_norm_g"].reshape(8, 128).T
    p["gains"] = g.reshape(128, -1)
    sc = np.zeros((128, L, 2, 4), np.float32)
    for l in range(L):
        for ch in range(2):
            sc[:, l, ch, 0:3] = inp["sc_conv_w"][l][:, ch * 128:(ch + 1) * 128].T
            sc[:, l, ch, 3] = inp["sc_conv_b"][l][ch * 128:(ch + 1) * 128]
    p["scw"] = sc.reshape(128, -1)
    fc = np.zeros((128, L, 44, 4), np.float32)
    for l in range(L):
        for half in range(2):
            for jc in range(22):
                lo = jc * 128
                n = min(128, DFF - lo)
                cols = slice(half * DFF + lo, half * DFF + lo + n)
                fc[:n, l, half * 22 + jc, 0:3] = inp["ffn_conv_w"][l][:, cols].T
                fc[:n, l, half * 22 + jc, 3] = inp["ffn_conv_b"][l][cols]
    p["fcw"] = fc.reshape(128, -1)
    wg = np.zeros((32, L, 128), np.float32)
    for l in range(L):
        wg[0:16, l, :] = inp["gla_w_gate"][l]
        wg[16, l, :] = inp["gla_b_gate"][l]
    p["wg_aug"] = wg.reshape(32, -1)
    p["gn"] = np.ascontiguousarray(inp["gla_norm_g"].T)
    return p


def build(n_layers=L_ALL, debug=False, max_phase=9):
    nc = bass.Bass("TRN2", target_bir_lowering=False)
    I = {}
    for nm, shp in list(PARAM_SHAPES.items()) + list(CONST_SHAPES.items()):
        I[nm] = nc.dram_tensor(nm, shp, F32, kind="ExternalInput").ap()
    outT = nc.dram_tensor("outT", [D, T], F32, kind="ExternalOutput").ap()

    def scratch(nm, shp, dt):
        return nc.dram_tensor(nm, shp, dt, kind="Internal").ap()
    xs = scratch("xs", [8, 128, T], F32)
    q_d = scratch("q_d", [8, 64, T], BF16)
    k_d = scratch("k_d", [8, 64, T], BF16)
    v_d = scratch("v_d", [32, 128, 512], BF16)
    yconv_d = scratch("yconv_d", [2, 128, T], BF16)
    ymoba_d = scratch("ymoba_d", [8, 64, T], BF16)
    ygla_d = scratch("ygla_d", [4, 64, T], BF16)
    qd_d = scratch("qd_d", [128, T], BF16)
    kd_d = scratch("kd_d", [128, T], BF16)
    kend_d = scratch("kend_d", [32, 128, 512], BF16)
    vg_d = scratch("vg_d", [32, 128, 256], BF16)
    sg_d = scratch("sg_d", [4, 64, T], BF16)
    dbg = {}
    if debug:
        dbg["yconv"] = nc.dram_tensor("dbg_yconv", [2, 128, T], BF16, kind="ExternalOutput").ap()
        dbg["ymoba"] = nc.dram_tensor("dbg_ymoba", [8, 64, T], BF16, kind="ExternalOutput").ap()
        dbg["ygla"] = nc.dram_tensor("dbg_ygla", [4, 64, T], BF16, kind="ExternalOutput").ap()
        dbg["x2"] = nc.dram_tensor("dbg_x2", [8, 128, T], F32, kind="ExternalOutput").ap()

    B_xs = bufs(8, "xs")
    B_q = [bufs(8, "q%d_" % h) for h in range(8)]
    B_k = [bufs(8, "k%d_" % h) for h in range(8)]
    B_v = bufs(32, "v")
    B_yconv = bufs(8, "yc")
    B_ymoba = [bufs(8, "ym%d_" % h) for h in range(8)]
    B_ygla = bufs(32, "yg")
    B_qd = bufs(8, "qd")
    B_kd = bufs(8, "kd")
    B_kend = bufs(32, "kend")
    B_vg = bufs(32, "vg")
    B_sg = bufs(8, "sg")

    with ExitStack() as top:
        sems = {e: top.enter_context(nc.semaphore("s_" + e)) for e in ENGS}
        dma_sems = {q: [top.enter_context(nc.semaphore("d_%s_%d" % (q, i))) for i in range(NDMA_SEM)] for q in DMAQ}
        P = Prog(nc, sems, dma_sems)

        uid = [0]

        def sbt(es, name, shape, dt):
            uid[0] += 1
            return es.enter_context(nc.sbuf_tensor("sb%d_%s" % (uid[0], name), shape, dt))

        gains = sbt(top, "gains", [128, (L_ALL * 4 + 1) * 8], F32)
        scw = sbt(top, "scw", [128, L_ALL * 2 * 4], F32)
        fcw = sbt(top, "fcw", [128, L_ALL * 44 * 4], F32)
        wg = sbt(top, "wg", [32, L_ALL * 128], F32)
        gn = sbt(top, "gn", [64, L_ALL], F32)
        hm = sbt(top, "hm", [128, 4], F32)
        ident = sbt(top, "ident", [128, 128], BF16)
        ones_bf = sbt(top, "ones_bf", [128, 128], BF16)
        ones_f = sbt(top, "ones_f", [128, 64], F32)
        dec = sbt(top, "dec", [128, 64], F32)
        B_dec = Buf("dec")
        B_const = Buf("const")
        banks = [top.enter_context(nc.psum_tensor("bank%d" % i, [128, 512], F32)) for i in range(8)]
        B_bank = bufs(8, "bank")

        class Rot:
            def __init__(self, ids):
                self.ids = ids
                self.i = 0

            def next(self):
                k = self.ids[self.i % len(self.ids)]
                self.i += 1
                return banks[k], B_bank[k]

        def MM(out, lhsT, rhs, start, stop, reads, writes, **kw):
            P.op("pe", lambda e: e.matmul(out, lhsT=lhsT, rhs=rhs, start=start, stop=stop, **kw), reads, writes)

        def ACT(out, in_, func, reads, writes, **kw):
            P.op("act", lambda e: e.activation(out=out, in_=in_, func=func, **kw), reads, writes)

        def TT(out, in0, in1, op, reads, writes, eng="dve"):
            P.op(eng, lambda e: e.tensor_tensor(out=out, in0=in0, in1=in1, op=op), reads, writes)

        def STT(out, in0, scalar, in1, op0, op1, reads, writes):
            P.op("dve", lambda e: e.scalar_tensor_tensor(out=out, in0=in0, scalar=scalar, in1=in1, op0=op0, op1=op1), reads, writes)

        def TS(out, in0, s1, s2, op0, op1, reads, writes, eng="dve"):
            if op1 is None:
                P.op(eng, lambda e: e.tensor_scalar(out=out, in0=in0, scalar1=s1, scalar2=None, op0=op0), reads, writes)
            else:
                P.op(eng, lambda e: e.tensor_scalar(out=out, in0=in0, scalar1=s1, scalar2=s2, op0=op0, op1=op1), reads, writes)

        def CP(out, in_, reads, writes, eng="dve"):
            P.op(eng, lambda e: e.tensor_copy(out=out, in_=in_), reads, writes)

        def MEMSET(ap, val, writes, eng="dve"):
            P.op(eng, lambda e: e.memset(ap, val), (), writes)

        def DMA(q, out, in_, reads, writes):
            P.dma(q, lambda e: e.dma_start(out=out, in_=in_), reads, writes)

        def gcol(l, kind, c):
            i = ((l * 4 + kind) * 8 + c) if l < L_ALL else (L_ALL * 4 * 8 + c)
            return gains[:, i:i + 1]

        DMA("sp", gains[:], I["gains"], (), [B_const])
        DMA("sp", scw[:], I["scw"], (), [B_const])
        DMA("sp", fcw[:], I["fcw"], (), [B_const])
        DMA("sp", wg[:], I["wg_aug"], (), [B_const])
        DMA("sp", gn[:], I["gn"], (), [B_const])
        DMA("sp", hm[:], I["hm"], (), [B_const])
        DMA("pool", ident[:], I["ident"], (), [B_const])
        MEMSET(ones_bf[:], 1.0, [B_const])
        MEMSET(ones_f[:], 1.0, [B_const])
        P.flush()

        def rmsnorm_tile(xt, Bx, ntok, l, kind, sq, Bsq, rstd, Brstd, hout, Bh, rot):
            ACT(sq[:, :, 0:ntok], xt[:, :, 0:ntok], AF.Square, [Bx], [Bsq])
            bk, Bb = rot.next()
            for c in range(8):
                MM(bk[:, 0:ntok], ones_bf[:], sq[:, c, 0:ntok], c == 0, c == 7, [Bsq, B_const], [Bb])
            ACT(rstd[:, 0:ntok], bk[:, 0:ntok], AF.Ln, [Bb], [Brstd], scale=1.0 / D, bias=EPS)
            ACT(rstd[:, 0:ntok], rstd[:, 0:ntok], AF.Exp, [Brstd], [Brstd], scale=-0.5)
            for c in range(8):
                STT(hout[:, c, 0:ntok], xt[:, c, 0:ntok], gcol(l, kind, c), rstd[:, 0:ntok], ALU.mult, ALU.mult,
                    [Bx, Brstd, B_const], [Bh[c]])

        def softmax_norm(po, Bpo, ydst, By, es_bufs, rot_bc):
            lnd, rden, bc_sb, Bn = es_bufs
            ACT(lnd[64:65, :], po[64:65, :], AF.Ln, [Bpo], [Bn[0]])
            ACT(rden[64:65, :], lnd[64:65, :], AF.Exp, [Bn[0]], [Bn[1]], scale=-1.0)
            bk, Bb = rot_bc.next()
            MM(bk[0:64, :], ones_f[64:65, 0:64], rden[64:65, :], True, True, [Bn[1], B_const], [Bb])
            ACT(bc_sb[0:64, :], bk[0:64, :], AF.Copy, [Bb], [Bn[2]])
            TT(ydst, po[0:64, :], bc_sb[0:64, :], ALU.mult, [Bpo, Bn[2]], By)

        for l in range(n_layers):
            xsrc = (lambda c, t0, n: I["xT"][c * 128:(c + 1) * 128, t0:t0 + n]) if l == 0 else None
            with ExitStack() as es:
                win = sbt(es, "win", [128, 8, 3088], BF16)
                B_win = Buf("win")
                xt2 = [sbt(es, "xt%d" % i, [128, 8, 512], F32) for i in range(2)]
                B_xt2 = bufs(2, "xt")
                sq = sbt(es, "sq", [128, 8, 512], BF16); B_sq = Buf("sq")
                rstd = sbt(es, "rstd", [128, 512], F32); B_rstd = Buf("rstd")
                hh2 = [sbt(es, "hh%d" % i, [128, 8, 512], BF16) for i in range(2)]; B_hh2 = [bufs(8, "hh%d_" % i) for i in range(2)]
                hh = hh2[0]; B_hh = B_hh2[0]
                tmpc = sbt(es, "tmpc", [128, 512], F32); B_tmpc = Buf("tmpc")
                ubuf = [sbt(es, "ubuf%d" % i, [128, 514], F32) for i in range(2)]; B_ubuf = bufs(2, "ubuf")
                c1 = sbt(es, "c1", [128, 512], F32); B_c1 = Buf("c1")
                ytile = sbt(es, "ytile", [128, 2, 512], BF16); B_ytile = bufs(2, "ytile")
                qst = sbt(es, "qst", [128, 4, 512], BF16); B_qst = bufs(4, "qst")
                kst = sbt(es, "kst", [128, 4, 512], BF16); B_kst = bufs(4, "kst")
                vst = sbt(es, "vst", [128, 4, 512], BF16); B_vst = bufs(4, "vst")
                vgst = sbt(es, "vgst", [128, 4, 256], BF16); B_vgst = bufs(4, "vgst")
                kendst = sbt(es, "kendst", [128, 4, 640], BF16); B_kendst = bufs(4, "kendst")
                sgst = sbt(es, "sgst", [128, 2, 512], BF16); B_sgst = bufs(2, "sgst")
                glr = sbt(es, "glr", [32, 512], F32); B_glr = Buf("glr")
                e1 = sbt(es, "e1", [128, 128], F32); B_e1 = Buf("e1")
                spl = sbt(es, "spl", [128, 128], F32); B_spl = Buf("spl")
                E1 = sbt(es, "E1", [128, 512], F32); B_E1 = bufs(4, "E1")
                E2 = sbt(es, "E2", [128, 512], F32); B_E2 = bufs(4, "E2")
                E3 = sbt(es, "E3", [128, 128], F32); B_E3 = Buf("E3")
                ltri = sbt(es, "ltri", [128, 128], F32)
                lrev = sbt(es, "lrev", [128, 128], F32)
                qdst = sbt(es, "qdst", [128, 512], BF16); B_qdst = Buf("qdst")
                kdst = sbt(es, "kdst", [128, 512], BF16); B_kdst = Buf("kdst")
                B_c = Buf("p1const")
                rot = Rot([0, 1, 2, 3, 4, 5])
                rot_hold = Rot([6, 7])

                win_src = I["w_in"][l].rearrange("(c p) n -> p c n", p=128)
                B_winh = bufs(2, "winh")
                for hi, (a, b) in enumerate([(0, 1544), (1544, 3088)]):
                    DMA("pool", win[:, :, a:b], win_src[:, :, a:b], (), [B_winh[hi]])

                def winb(c0, c1):
                    r = []
                    if c0 < 1544:
                        r.append(B_winh[0])
                    if c1 > 1544:
                        r.append(B_winh[1])
                    return r
                DMA("sp", ltri[:], I["ltri"], (), [B_c])
                DMA("sp", lrev[:], I["lrev"], (), [B_c])
                MEMSET(ubuf[0][:, 0:2], 0.0, [B_ubuf[0]])
                MEMSET(ubuf[1][:, 0:2], 0.0, [B_ubuf[1]])
                MEMSET(glr[:], 1.0, [B_glr])
                MEMSET(kendst[:], 0.0, B_kendst)
                scl = lambda ch, k: scw[:, (l * 2 + ch) * 4 + k:(l * 2 + ch) * 4 + k + 1]

                def proj(out, cols, rows_rhs, reads_extra, Bb, M=None):
                    for c in range(8):
                        MM(out, win[:, c, cols[0]:cols[1]], hh[:, c, :], c == 0, c == 7, winb(cols[0], cols[1]) + [B_hh[c]], [Bb])

                def load_norm(tt):
                    t0 = tt * 512
                    xt = xt2[tt % 2]; Bx = B_xt2[tt % 2]
                    if l == 0:
                        DMA("sp", xt[:], I["xT"].rearrange("(c p) t -> p c t", p=128)[:, :, t0:t0 + 512], (), [Bx])
                    else:
                        DMA("sp", xt[:], xs.rearrange("c p t -> p c t")[:, :, t0:t0 + 512], [B_xs[tt]], [Bx])
                    rmsnorm_tile(xt, Bx, 512, l, 0, sq, B_sq, rstd, B_rstd, hh2[tt % 2], B_hh2[tt % 2], rot)

                load_norm(0)
                for tt in range(8):
                    t0 = tt * 512
                    hh = hh2[tt % 2]; B_hh = B_hh2[tt % 2]
                    for ch in range(2):
                        pc, Bpc = rot.next()
                        proj(pc[:], (256 + 128 * ch, 384 + 128 * ch), None, None, Bpc)
                        ACT(tmpc[:], pc[:], AF.Copy, [Bpc], [B_tmpc])
                        ph, Bph = rot.next()
                        proj(ph[:], (512 + 128 * ch, 640 + 128 * ch), None, None, Bph)
                        TT(ubuf[ch][:, 2:514], ph[:], tmpc[:], ALU.mult, [Bph, B_tmpc], [B_ubuf[ch]])
                        ACT(c1[:], ubuf[ch][:, 2:514], AF.Identity, [B_ubuf[ch], B_const], [B_c1], scale=scl(ch, 2), bias=scl(ch, 3))
                        STT(c1[:], ubuf[ch][:, 1:513], scl(ch, 1), c1[:], ALU.mult, ALU.add, [B_ubuf[ch], B_c1, B_const], [B_c1])
                        STT(c1[:], ubuf[ch][:, 0:512], scl(ch, 0), c1[:], ALU.mult, ALU.add, [B_ubuf[ch], B_c1, B_const], [B_c1])
                        CP(ubuf[ch][:, 0:2], ubuf[ch][:, 512:514], [B_ubuf[ch]], [B_ubuf[ch]])
                        pb, Bpb = rot.next()
                        proj(pb[:], (128 * ch, 128 * ch + 128), None, None, Bpb)
                        TT(ytile[:, ch, :], pb[:], c1[:], ALU.mult, [Bpb, B_c1], [B_ytile[ch]])
                    DMA("pool", yconv_d.rearrange("c p t -> p c t")[:, :, t0:t0 + 512], ytile[:], B_ytile, [B_yconv[tt]])
                    for hp in range(4):
                        pq, Bpq = rot.next()
                        proj(pq[:], (768 + 128 * hp, 896 + 128 * hp), None, None, Bpq)
                        ACT(qst[:, hp, :], pq[:], AF.Identity, [Bpq], [B_qst[hp]], scale=0.125)
                        for two in range(2):
                            h = 2 * hp + two
                            DMA("pool", q_d[h][:, t0:t0 + 512], qst[64 * two:64 * two + 64, hp, :], [B_qst[hp]], [B_q[h][tt]])
                        pk, Bpk = rot.next()
                        proj(pk[:], (1280 + 128 * hp, 1408 + 128 * hp), None, None, Bpk)
                        CP(kst[:, hp, :], pk[:], [Bpk], [B_kst[hp]])
                        for two in range(2):
                            h = 2 * hp + two
                            DMA("pool", k_d[h][:, t0:t0 + 512], kst[64 * two:64 * two + 64, hp, :], [B_kst[hp]], [B_k[h][tt]])
                    if tt + 1 < 8:
                        load_norm(tt + 1)
                    for sub in range(4):
                        pv, Bpv = rot.next()
                        for c in range(8):
                            MM(pv[:], hh[:, c, sub * 128:(sub + 1) * 128], win[:, c, 1792:2304], c == 0, c == 7,
                               [B_winh[1], B_hh[c]], [Bpv])
                        if sub % 2 == 0:
                            ACT(vst[:, sub, :], pv[:], AF.Copy, [Bpv], [B_vst[sub]])
                        else:
                            CP(vst[:, sub, :], pv[:], [Bpv], [B_vst[sub]])
                        DMA("pool", v_d[tt * 4 + sub], vst[:, sub, :], [B_vst[sub]], [B_v[tt * 4 + sub]])
                    pgq, Bpgq = rot_hold.next()
                    proj(pgq[:], (2304, 2432), None, None, Bpgq)
                    pgk, Bpgk = rot_hold.next()
                    proj(pgk[:], (2432, 2560), None, None, Bpgk)
                    plr, Bplr = rot.next()
                    proj(plr[0:16, :], (3072, 3088), None, None, Bplr)
                    ACT(glr[0:16, :], plr[0:16, :], AF.Copy, [Bplr], [B_glr])
                    for sub in range(4):
                        kt = tt * 4 + sub
                        pkv, Bpkv = rot.next()
                        for c in range(8):
                            MM(pkv[:, 0:384], hh[:, c, sub * 128:(sub + 1) * 128], win[:, c, 2432:2816], c == 0, c == 7,
                               [B_winh[1], B_hh[c]], [Bpkv])
                        CP(vgst[:, sub, :], pkv[:, 128:384], [Bpkv], [B_vgst[sub]])
                        DMA("pool", vg_d[kt], vgst[:, sub, :], [B_vgst[sub]], [B_vg[kt]])
                        pla, Bpla = rot.next()
                        MM(pla[:, 0:128], glr[0:32, sub * 128:(sub + 1) * 128], wg[0:32, l * 128:(l + 1) * 128], True, True,
                           [B_glr, B_const], [Bpla])
                        ACT(e1[:], pla[:, 0:128], AF.Exp, [Bpla], [B_e1], scale=-1.0)
                        ACT(spl[:], e1[:], AF.Ln, [B_e1], [B_spl], bias=1.0)
                        pbt, Bpbt = rot.next()
                        MM(pbt[:, 0:128], spl[:], ltri[:], True, True, [B_spl, B_c], [Bpbt])
                        MM(pbt[:, 128:256], lrev[:], spl[:], True, True, [B_spl, B_c], [Bpbt])
                        ACT(E1[:, sub * 128:(sub + 1) * 128], pbt[:, 0:128], AF.Exp, [Bpbt], [B_E1[sub]])
                        ACT(E2[:, sub * 128:(sub + 1) * 128], pbt[:, 0:128], AF.Exp, [Bpbt], [B_E2[sub]], scale=-1.0)
                        ACT(E3[:], pbt[:, 128:256], AF.Exp, [Bpbt], [B_E3])
                        TT(kendst[:, sub, :].rearrange("p (h x) -> p h x", x=160)[:, :, 0:32],
                           pkv[:, 0:128].rearrange("p (h x) -> p h x", x=32),
                           E3[:].rearrange("p (h x) -> p h x", x=32), ALU.mult, [Bpkv, B_E3], [B_kendst[sub]])
                        DMA("pool", kend_d[kt], kendst[:, sub, 0:512], [B_kendst[sub]], [B_kend[kt]])
                        CP(dec[:, 2 * kt:2 * kt + 2], E1[:, sub * 128 + 63:sub * 128 + 128:64], [B_E1[sub]], [B_dec])
                    STT(qdst[:], pgq[:], 32.0 ** -0.5, E1[:], ALU.mult, ALU.mult, [Bpgq] + B_E1, [B_qdst])
                    DMA("pool", qd_d[:, t0:t0 + 512], qdst[:], [B_qdst], [B_qd[tt]])
                    TT(kdst[:], pgk[:], E2[:], ALU.mult, [Bpgk] + B_E2, [B_kdst])
                    DMA("pool", kd_d[:, t0:t0 + 512], kdst[:], [B_kdst], [B_kd[tt]])
                    for hp in range(2):
                        pgo, Bpgo = rot.next()
                        proj(pgo[:], (2816 + 128 * hp, 2944 + 128 * hp), None, None, Bpgo)
                        ACT(sgst[:, hp, :], pgo[:], AF.Silu, [Bpgo], [B_sgst[hp]])
                    DMA("pool", sg_d.rearrange("(hp two) p t -> (two p) hp t", two=2)[:, :, t0:t0 + 512], sgst[:], B_sgst, [B_sg[tt]])
                P.flush()

            if max_phase < 2:
                break
            with ExitStack() as es:
                QA = [sbt(es, "QA%d" % i, [96, T], BF16) for i in range(2)]
                KA = [sbt(es, "KA%d" % i, [96, T], BF16) for i in range(2)]
                VA = [sbt(es, "VA%d" % i, [128, 32, 65], BF16) for i in range(2)]
                B_QAm = bufs(2, "QAm"); B_KA = bufs(2, "KA"); B_VA = bufs(2, "VA")
                B_QAmask = [bufs(8, "QAmask%d_" % i) for i in range(2)]
                cm = sbt(es, "cm", [128, 4, 512], BF16)
                pmt = sbt(es, "pmt", [128, 32, 16], F32)
                ownfix = sbt(es, "ownfix", [128, 32, 16], F32)
                alibase = sbt(es, "alibase", [128, 32, 16], F32)
                B_c = Buf("p2const")
                km32 = sbt(es, "km32", [64, 16], F32); B_km32 = Buf("km32")
                kmb2 = [sbt(es, "kmb%d" % i, [64, 16], BF16) for i in range(2)]; B_kmb2 = bufs(2, "kmb")
                gm = sbt(es, "gm", [128, 4, 16], F32); B_gm = Buf("gm")
                top8 = sbt(es, "top8", [128, 4, 8], F32); B_top8 = Buf("top8")
                sel = sbt(es, "sel", [128, 4, 16], F32); B_sel = Buf("sel")
                stage = sbt(es, "stage", [128, 4, 128], BF16); B_stage = Buf("stage")
                pt = [sbt(es, "pt%d" % i, [128, 512], BF16) for i in range(6)]; B_pt = bufs(6, "pt")
                lnd = sbt(es, "lnd", [128, 512], F32)
                rden = lnd
                bc_sb = sbt(es, "bc_sb", [64, 512], F32)
                B_n = bufs(3, "nrm")
                B_n[1] = B_n[0]
                yst = [sbt(es, "yst%d" % i, [64, 512], BF16) for i in range(2)]; B_yst = bufs(2, "yst")
                rot_s = Rot([0, 1, 2, 3, 6])
                rot_o = Rot([4, 5])
                rot_g = Rot([7])
                rot_t = Rot([7])

                DMA("pool", cm[:].rearrange("p a b -> p (a b)"), I["cm"], (), [B_c])
                DMA("sp", pmt[:].rearrange("p a b -> p (a b)"), I["pm"], (), [B_c])
                DMA("sp", ownfix[:].rearrange("p a b -> p (a b)"), I["ownfix"], (), [B_c])
                DMA("sp", alibase[:].rearrange("p a b -> p (a b)"), I["alibase"], (), [B_c])
                MEMSET(stage[:], 0.0, [B_stage])
                for i in range(2):
                    for hf in range(2):
                        DMA("pool", QA[i][80:96, hf * 2048:(hf + 1) * 2048], I["qstat"][:, hf * 2048:(hf + 1) * 2048], (), [B_QAm[i]])
                    MEMSET(VA[i][:, :, 64:65], 1.0, [B_VA[i]])
                ycnt = [0]

                def prologue(h):
                    bi = h % 2
                    DMA("sp", QA[bi][0:64, :], q_d[h], B_q[h], [B_QAm[bi]])
                    DMA("sp", KA[bi][0:64, :], k_d[h], B_k[h], [B_KA[bi]])
                    for hf in range(2):
                        DMA("pool", KA[bi][64:96, hf * 2048:(hf + 1) * 2048], I["kstat"][h][:, hf * 2048:(hf + 1) * 2048], (), [B_KA[bi]])
                    DMA("sp", VA[bi][:, :, 0:64], v_d.rearrange("k p (h e) -> p k h e", e=64)[:, :, h, :], B_v, [B_VA[bi]])

                def kmean(h):
                    bi = h % 2
                    P.op("dve", lambda e, bi=bi: e.tensor_reduce(out=km32[:], in_=KA[bi][0:64, :].rearrange("p (j k) -> p j k", k=256),
                                                                axis=AX.X, op=ALU.add), [B_KA[bi]], [B_km32])
                    ACT(kmb2[bi][:], km32[:], AF.Identity, [B_km32], [B_kmb2[bi]], scale=1.0 / 256)

                def gate_mm(h, g):
                    bi = h % 2
                    slope = 2.0 ** (-(h + 1))
                    pg, Bpg = rot_g.next()
                    for qt in range(4):
                        q0 = g * 512 + qt * 128
                        MM(pg[:, qt * 16:(qt + 1) * 16], QA[bi][0:64, q0:q0 + 128], kmb2[bi][:], True, True,
                           [B_QAm[bi], B_kmb2[bi]], [Bpg])
                    TT(gm[:], pg[:, 0:64].rearrange("p (a b) -> p a b", b=16), pmt[:, 4 * g:4 * g + 4, :], ALU.add,
                       [Bpg, B_c], [B_gm])
                    for qt in range(4):
                        P.op("dve", lambda e, qt=qt: e.max(out=top8[:, qt, :], in_=gm[:, qt, :]), [B_gm], [B_top8])
                    for qt in range(4):
                        TS(sel[:, qt, :], gm[:, qt, :], top8[:, qt, 2:3], None, ALU.is_ge, None, [B_gm, B_top8], [B_sel])
                    TS(sel[:], sel[:], -1.0, -NEG, ALU.add, ALU.mult, [B_sel], [B_sel])
                    TT(sel[:], sel[:], ownfix[:, 4 * g:4 * g + 4, :], ALU.max, [B_sel, B_c], [B_sel])
                    STT(stage[:, :, 64:80], alibase[:, 4 * g:4 * g + 4, :], slope, sel[:], ALU.mult, ALU.add,
                        [B_sel, B_c], [B_stage])

                def gate_T(h, g):
                    bi = h % 2
                    pT, BpT = rot_t.next()
                    for qt in range(4):
                        MM(pT[:, qt * 128:(qt + 1) * 128], stage[:, qt, :], ident[:], True, True, [B_stage, B_const], [BpT])
                    CP(QA[bi][64:80, g * 512:(g + 1) * 512], pT[64:80, :], [BpT], [B_QAmask[bi][g]])

                NPT = len(pt)
                LOOK = 4
                pend_norm = []

                def attention(h, g):
                    bi = h % 2
                    nkt = 4 * (g + 1)
                    po, Bpo = rot_o.next()
                    qs = QA[bi][0:96, g * 512:(g + 1) * 512]

                    def S_tile(kt):
                        pS, BpS = rot_s.next()
                        diag = kt >= 4 * g
                        MM(pS[:], KA[bi][0:96, kt * 128:(kt + 1) * 128], qs, True, not diag,
                           [B_KA[bi], B_QAm[bi], B_QAmask[bi][g]], [BpS])
                        if diag:
                            MM(pS[:], ident[:], cm[:, kt - 4 * g, :], False, True, [B_const, B_c], [BpS])
                        j = kt % NPT
                        ACT(pt[j][:], pS[:], AF.Exp, [BpS], [B_pt[j]])

                    def PV_tile(kt):
                        j = kt % NPT
                        MM(po[0:65, :], VA[bi][:, kt, 0:65], pt[j][:], kt == 0, kt == nkt - 1, [B_VA[bi], B_pt[j]], [Bpo])
                    for kt in range(min(LOOK, nkt)):
                        S_tile(kt)
                    for kt in range(nkt):
                        if kt + LOOK < nkt:
                            S_tile(kt + LOOK)
                        PV_tile(kt)
                        if kt == 3 and pend_norm:
                            pend_norm.pop(0)()
                    yi = ycnt[0] % 2
                    ycnt[0] += 1
                    ACT(lnd[64:65, :], po[64:65, :], AF.Ln, [Bpo], [B_n[0]])
                    ACT(rden[64:65, :], lnd[64:65, :], AF.Exp, [B_n[0]], [B_n[1]], scale=-1.0)

                    def rest(po=po, Bpo=Bpo, yi=yi, h=h, g=g):
                        bk, Bb = rot_t.next()
                        MM(bk[0:64, :], ones_f[64:65, 0:64], rden[64:65, :], True, True, [B_n[1], B_const], [Bb])
                        CP(bc_sb[0:64, :], bk[0:64, :], [Bb], [B_n[2]])
                        TT(yst[yi][:], po[0:64, :], bc_sb[0:64, :], ALU.mult, [Bpo, B_n[2]], [B_yst[yi]])
                        DMA("pool", ymoba_d[h][:, g * 512:(g + 1) * 512], yst[yi][:], [B_yst[yi]], [B_ymoba[h][g]])
                    pend_norm.append(rest)

                prologue(0)
                kmean(0)
                for g in range(8):
                    gate_mm(0, g)
                    gate_T(0, g)
                pending = []
                for h in range(8):
                    if h + 1 < 8:
                        prologue(h + 1)
                    for g in range(8):
                        if g == 5 and h + 1 < 8:
                            kmean(h + 1)
                            pending.extend((h + 1, gg) for gg in range(8))
                        task = pending.pop(0) if pending else None
                        if task:
                            gate_mm(*task)
                        attention(h, g)
                        if task:
                            gate_T(*task)
                assert not pending
                while pend_norm:
                    pend_norm.pop(0)()
                qdT = sbt(es, "qdT", [128, T], BF16); B_qdT = Buf("qdT")
                kdT = sbt(es, "kdT", [128, T], BF16); B_kdT = Buf("kdT")
                kend = sbt(es, "kend", [128, 32, 512], BF16); B_kendS = Buf("kendS")
                vg = sbt(es, "vg", [128, 32, 256], BF16); B_vgS = Buf("vgS")
                sg = sbt(es, "sg", [64, 4, T], BF16); B_sgS = Buf("sgS")
                Sall = sbt(es, "Sall", [128, 65, 64], F32); B_S = bufs(65, "S")
                Sbf = sbt(es, "Sbf", [128, 64, 64], BF16); B_Sbf = Buf("Sbf")
                tri01 = sbt(es, "tri01", [128, 512], BF16); B_c = Buf("p3const")
                qbd = [sbt(es, "qbd%d" % i, [128, 4, 128], BF16) for i in range(2)]; B_qbd = bufs(2, "qbd")
                Am = [sbt(es, "Am%d" % i, [128, 4, 128], BF16) for i in range(2)]; B_Am = bufs(2, "Am")
                sqg = sbt(es, "sqg", [64, 512], BF16); B_sqg = Buf("sqg")
                rs = sbt(es, "rs", [64, 512], F32); B_rs = Buf("rs")
                t1 = sbt(es, "t1", [64, 512], F32); B_t1 = Buf("t1")
                ygst = [sbt(es, "ygst%d" % i, [64, 4, 128], BF16) for i in range(2)]; B_ygst = bufs(2, "ygst")
                rot_u = Rot([0, 1])
                rot_a = Rot([2, 3])
                rot_o = Rot([4, 5])
                rot_s = Rot([6, 7])

                DMA("pool", tri01[:], I["tri01"], (), [B_c])
                DMA("sp", qdT[:], qd_d, B_qd, [B_qdT])
                DMA("sp", kdT[:], kd_d, B_kd, [B_kdT])
                for part in range(4):
                    DMA("sp", kend[:, part * 8:(part + 1) * 8, :], kend_d.rearrange("k p x -> p k x")[:, part * 8:(part + 1) * 8, :],
                        B_kend[part * 8:(part + 1) * 8], [B_kendS])
                    DMA("sp", vg[:, part * 8:(part + 1) * 8, :], vg_d.rearrange("k p x -> p k x")[:, part * 8:(part + 1) * 8, :],
                        B_vg[part * 8:(part + 1) * 8], [B_vgS])
                DMA("sp", sg[:], sg_d.rearrange("h p t -> p h t"), B_sg, [B_sgS])
                MEMSET(Sall[:, 0, :], 0.0, [B_S[0]])
                rot_ue = Rot([0, 1])
                rot_uo = Rot([6, 7])
                for grp in range(4):
                    pUe, BpUe = rot_ue.next()
                    pUo, BpUo = rot_uo.next()
                    for n in range(16):
                        N = grp * 16 + n
                        kt = N // 2
                        p0 = 64 * (N % 2)
                        pU, BpU = (pUe, BpUe) if N % 2 == 0 else (pUo, BpUo)
                        cs = slice((n // 2) * 64, (n // 2) * 64 + 64)
                        for h in range(4):
                            MM(pU[:, cs], kend[p0:p0 + 64, kt, h * 128:(h + 1) * 128],
                               vg[p0:p0 + 64, kt, h * 64:(h + 1) * 64], h == 0, h == 3, [B_kendS, B_vgS], [BpU])
                    for n in range(16):
                        N = grp * 16 + n
                        pU, BpU = (pUe, BpUe) if N % 2 == 0 else (pUo, BpUo)
                        cs = slice((n // 2) * 64, (n // 2) * 64 + 64)
                        STT(Sall[:, N + 1, :], Sall[:, N, :], dec[:, N:N + 1], pU[:, cs], ALU.mult, ALU.add,
                            [B_S[N], B_dec, BpU], [B_S[N + 1]])
                ACT(Sbf[:].rearrange("p a b -> p (a b)"), Sall[:, 0:64, :].rearrange("p a b -> p (a b)"), AF.Copy, B_S[0:64], [B_Sbf])
                st3 = {}
                rs2 = [rs, sbt(es, "rs_b", [64, 512], F32)]; B_rs2 = [B_rs, Buf("rs_b")]
                t12 = [t1, sbt(es, "t1_b", [64, 512], F32)]; B_t12 = [B_t1, Buf("t1_b")]
                sqg2 = [sqg, sqg]; B_sqg2 = [B_sqg, B_sqg]

                def g1(kt):
                    i2 = kt % 2
                    tok = slice(kt * 128, (kt + 1) * 128)
                    for h in range(4):
                        TS(qbd[i2][:, h, :], qdT[:, tok], hm[:, h:h + 1], None, ALU.mult, None, [B_qdT, B_const], [B_qbd[i2]])
                    pA, BpA = rot_a.next()
                    MM(pA[:], kdT[:, tok], qbd[i2][:].rearrange("p a b -> p (a b)"), True, True, [B_kdT, B_qbd[i2]], [BpA])
                    TT(Am[i2][:].rearrange("p a b -> p (a b)"), pA[:], tri01[:], ALU.mult, [BpA, B_c], [B_Am[i2]])

                def g2(kt):
                    i2 = kt % 2
                    po, Bpo = rot_o.next()
                    for half in range(2):
                        for h in range(4):
                            MM(po[0:64, h * 128 + 64 * half:h * 128 + 64 * half + 64], Sbf[:, 2 * kt + half, :],
                               qbd[i2][:, h, 64 * half:64 * half + 64],
                               half == 0 and h == 0, False, [B_Sbf, B_qbd[i2]], [Bpo], skip_group_check=True)
                    for h in range(4):
                        MM(po[0:64, h * 128:(h + 1) * 128], vg[:, kt, h * 64:(h + 1) * 64], Am[i2][:, h, :], False, h == 3,
                           [B_vgS, B_Am[i2]], [Bpo], skip_group_check=True)
                    ACT(sqg2[i2][:], po[0:64, :], AF.Square, [Bpo], [B_sqg2[i2]])
                    pss, Bpss = rot_s.next()
                    MM(pss[0:64, :], ones_bf[0:64, 0:64], sqg2[i2][:], True, True, [B_sqg2[i2], B_const], [Bpss])
                    st3[kt] = (po, Bpo, pss, Bpss)

                def g3(kt):
                    i2 = kt % 2
                    tok = slice(kt * 128, (kt + 1) * 128)
                    po, Bpo, pss, Bpss = st3.pop(kt)
                    ACT(rs2[i2][:], pss[0:64, :], AF.Ln, [Bpss], [B_rs2[i2]], scale=1.0 / 64, bias=EPS)
                    ACT(rs2[i2][:], rs2[i2][:], AF.Exp, [B_rs2[i2]], [B_rs2[i2]], scale=-0.5)
                    STT(t12[i2][:], po[0:64, :], gn[:, l:l + 1], rs2[i2][:], ALU.mult, ALU.mult, [Bpo, B_rs2[i2], B_const], [B_t12[i2]])
                    TT(ygst[i2][:], t12[i2][:].rearrange("p (h c) -> p h c", c=128), sg[:, :, tok], ALU.mult, [B_t12[i2], B_sgS], [B_ygst[i2]])
                    DMA("pool", ygla_d.rearrange("h p t -> p h t")[:, :, tok], ygst[i2][:], [B_ygst[i2]], [B_ygla[kt]])

                for step in range(32 + 2):
                    if step < 32:
                        g1(step)
                    if 0 <= step - 1 < 32:
                        g2(step - 1)
                    if 0 <= step - 2 < 32:
                        g3(step - 2)
                P.flush()

            if max_phase < 4:
                if debug:
                    DMA("sp", dbg["yconv"], yconv_d, B_yconv, [Buf()])
                    DMA("sp", dbg["ymoba"], ymoba_d, [b for hb in B_ymoba for b in hb], [Buf()])
                    DMA("sp", dbg["ygla"], ygla_d, B_ygla, [Buf()])
                    P.flush()
                break
            with ExitStack() as es:
                wo_c = sbt(es, "wo_c", [128, 2, D], BF16)
                wo_m = sbt(es, "wo_m", [128, 4, D], BF16)
                wo_g = sbt(es, "wo_g", [128, 2, D], BF16)
                wq = sbt(es, "wq", [128, 8, 256], BF16)
                wkv = sbt(es, "wkv", [128, 8, 512], BF16)
                wo_x = sbt(es, "wo_x", [128, 4, D], BF16)
                B_w = Buf("p4w")
                memt = sbt(es, "memt", [128, 8, 256], F32); B_memt = Buf("memt")
                memn = sbt(es, "memn", [128, 8, 256], BF16); B_memn = bufs(8, "memn")
                KmT = sbt(es, "KmT", [128, 4, 256], BF16); B_KmT = Buf("KmT")
                Vm = sbt(es, "Vm", [128, 2, 4, 65], BF16); B_Vm = Buf("Vm")
                xt2 = [sbt(es, "xt%d" % i, [128, 8, 512], F32) for i in range(2)]; B_xt2 = [bufs(8, "xt%d_" % i) for i in range(2)]
                B_xtall = bufs(2, "xtall")
                yc = sbt(es, "yc", [128, 2, 512], BF16); B_yc = Buf("yc")
                ym = sbt(es, "ym", [128, 4, 512], BF16); B_ym = Buf("ym")
                yg = sbt(es, "yg", [128, 2, 512], BF16); B_yg = Buf("yg")
                sq = sbt(es, "sq", [128, 8, 512], BF16); B_sq = Buf("sq")
                rstd = sbt(es, "rstd", [128, 512], F32); B_rstd = Buf("rstd")
                hh = sbt(es, "hh", [128, 8, 512], BF16); B_hh = bufs(8, "hh")
                qx4 = [sbt(es, "qx%d" % i, [128, 512], BF16) for i in range(4)]; B_qx4 = bufs(4, "qx")
                pt8 = [sbt(es, "pt%d" % i, [128, 512], BF16) for i in range(8)]; B_pt8 = bufs(8, "pt")
                ox = sbt(es, "ox", [128, 4, 512], BF16); B_ox = bufs(4, "ox")
                nset = [(sbt(es, "lnd%d" % i, [128, 512], F32), sbt(es, "rden%d" % i, [128, 512], F32),
                         sbt(es, "bc_sb%d" % i, [64, 512], F32), bufs(3, "nrm%d_" % i)) for i in range(2)]
                rot = Rot([0, 1, 2, 3, 4])
                rot_o = Rot([5, 6])
                rot_t = Rot([7])

                wsrc = I["w_out"][l]
                DMA("pool", wo_c[:], wsrc[0:256].rearrange("(c p) n -> p c n", p=128), (), [B_w])
                DMA("pool", wo_m[:], wsrc[256:768].rearrange("(h p) n -> p h n", p=128), (), [B_w])
                DMA("pool", wo_g[:], wsrc[768:1024].rearrange("(h p) n -> p h n", p=128), (), [B_w])
                DMA("pool", wq[:], I["xattn_wq"][l].rearrange("(c p) n -> p c n", p=128), (), [B_w])
                DMA("pool", wkv[:], I["xattn_wkv"][l].rearrange("(c p) n -> p c n", p=128), (), [B_w])
                MEMSET(wo_x[64:128, :, :], 0.0, [B_w])
                MEMSET(KmT[64:128, :, :], 0.0, [B_KmT])
                MEMSET(ox[64:128, :, :], 0.0, B_ox)
                for i in range(4):
                    MEMSET(qx4[i][64:128, :], 0.0, [B_qx4[i]])
                DMA("pool", wo_x[0:64, :, :], I["xattn_wo"][l].rearrange("(h p) n -> p h n", p=64), (), [B_w])
                DMA("sp", memt[:], I["memT"].rearrange("(c p) t -> p c t", p=128), (), [B_memt])
                MEMSET(Vm[:, :, :, 64:65], 1.0, [B_Vm])
                rmsnorm_tile(memt, B_memt, 256, l, 2, sq, B_sq, rstd, B_rstd, memn, B_memn, rot)
                for h in range(4):
                    pk, Bpk = rot.next()
                    for c in range(8):
                        MM(pk[0:64, 0:256], wkv[:, c, 64 * h:64 * h + 64], memn[:, c, :], c == 0, c == 7, [B_w, B_memn[c]], [Bpk])
                    CP(KmT[0:64, h, :], pk[0:64, 0:256], [Bpk], [B_KmT])
                for mt in range(2):
                    pv, Bpv = rot.next()
                    for c in range(8):
                        MM(pv[:, 0:256], memn[:, c, mt * 128:(mt + 1) * 128], wkv[:, c, 256:512], c == 0, c == 7, [B_w, B_memn[c]], [Bpv])
                    CP(Vm[:, mt, :, 0:64], pv[:, 0:256].rearrange("p (h e) -> p h e", e=64), [Bpv], [B_Vm])

                def Wst(tt):
                    t0 = tt * 512
                    xi = tt % 2
                    xt = xt2[xi]; Bx = B_xt2[xi]
                    if l == 0:
                        DMA("sp", xt[:], I["xT"].rearrange("(c p) t -> p c t", p=128)[:, :, t0:t0 + 512], (), Bx)
                    else:
                        DMA("sp", xt[:], xs.rearrange("c p t -> p c t")[:, :, t0:t0 + 512], [B_xs[tt]], Bx)
                    DMA("sp", yc[:], yconv_d.rearrange("c p t -> p c t")[:, :, t0:t0 + 512], [B_yconv[tt]], [B_yc])
                    DMA("sp", ym[:], ymoba_d.rearrange("(hp two) p t -> (two p) hp t", two=2)[:, :, t0:t0 + 512], [B_ymoba[h][tt] for h in range(8)], [B_ym])
                    DMA("sp", yg[:], ygla_d.rearrange("(hp two) p t -> (two p) hp t", two=2)[:, :, t0:t0 + 512], B_ygla[tt * 4:tt * 4 + 4], [B_yg])
                    for oc in range(8):
                        cs = slice(oc * 128, (oc + 1) * 128)
                        pw, Bpw = rot.next()
                        for c in range(2):
                            MM(pw[:], wo_c[:, c, cs], yc[:, c, :], c == 0, False, [B_w, B_yc], [Bpw])
                        for h in range(4):
                            MM(pw[:], wo_m[:, h, cs], ym[:, h, :], False, False, [B_w, B_ym], [Bpw])
                        for h in range(2):
                            MM(pw[:], wo_g[:, h, cs], yg[:, h, :], False, h == 1, [B_w, B_yg], [Bpw])
                        TT(xt[:, oc, :], pw[:], xt[:, oc, :], ALU.add, [Bpw, Bx[oc]], [Bx[oc]])

                def Nst(tt):
                    xi = tt % 2
                    xt = xt2[xi]; Bx = B_xt2[xi]
                    ACT(sq[:], xt[:], AF.Square, Bx, [B_sq])
                    bk, Bb = rot.next()
                    for c in range(8):
                        MM(bk[:], ones_bf[:], sq[:, c, :], c == 0, c == 7, [B_sq, B_const], [Bb])
                    ACT(rstd[:], bk[:], AF.Ln, [Bb], [B_rstd], scale=1.0 / D, bias=EPS)
                    ACT(rstd[:], rstd[:], AF.Exp, [B_rstd], [B_rstd], scale=-0.5)
                    for c in range(8):
                        STT(hh[:, c, :], xt[:, c, :], gcol(l, 1, c), rstd[:], ALU.mult, ALU.mult, [Bx[c], B_rstd, B_const], [B_hh[c]])

                def Xst(tt):
                    for h in range(4):
                        pq, Bpq = rot.next()
                        for c in range(8):
                            MM(pq[0:64, :], wq[:, c, 64 * h:64 * h + 64], hh[:, c, :], c == 0, c == 7, [B_w, B_hh[c]], [Bpq])
                        ACT(qx4[h][0:64, :], pq[0:64, :], AF.Identity, [Bpq], [B_qx4[h]], scale=0.125)
                    for h in range(4):
                        for mt in range(2):
                            pS, BpS = rot.next()
                            MM(pS[:], KmT[:, h, mt * 128:(mt + 1) * 128], qx4[h][:], True, True, [B_KmT, B_qx4[h]], [BpS])
                            ACT(pt8[h * 2 + mt][:], pS[:], AF.Exp, [BpS], [B_pt8[h * 2 + mt]])
                    for h in range(4):
                        po, Bpo = rot_o.next()
                        for mt in range(2):
                            MM(po[0:65, :], Vm[:, mt, h, 0:65], pt8[h * 2 + mt][:], mt == 0, mt == 1, [B_Vm, B_pt8[h * 2 + mt]], [Bpo])
                        softmax_norm(po, Bpo, ox[0:64, h, :], [B_ox[h]], nset[h % 2], rot_t)

                def Ost(tt):
                    t0 = tt * 512
                    xi = tt % 2
                    xt = xt2[xi]; Bx = B_xt2[xi]
                    for oc in range(8):
                        cs = slice(oc * 128, (oc + 1) * 128)
                        pw, Bpw = rot.next()
                        for h in range(4):
                            MM(pw[:], wo_x[:, h, cs], ox[:, h, :], h == 0, h == 3, [B_w, B_ox[h]], [Bpw])
                        TT(xt[:, oc, :], pw[:], xt[:, oc, :], ALU.add, [Bpw, Bx[oc]], [Bx[oc]])
                    DMA("pool", xs.rearrange("c p t -> p c t")[:, :, t0:t0 + 512], xt[:], Bx, [B_xs[tt]])

                Wst(0)
                for tt in range(8):
                    Nst(tt)
                    if tt + 1 < 8:
                        Wst(tt + 1)
                    Xst(tt)
                    Ost(tt)
                P.flush()

            if debug and l == 0:
                DMA("sp", dbg["yconv"], yconv_d, B_yconv, [Buf()])
                DMA("sp", dbg["ymoba"], ymoba_d, [b for hb in B_ymoba for b in hb], [Buf()])
                DMA("sp", dbg["ygla"], ygla_d, B_ygla, [Buf()])
                DMA("sp", dbg["x2"], xs, B_xs, [Buf()])
                P.flush()

            if max_phase < 6:
                break
            with ExitStack() as es:
                NTK = 256
                NT6 = T // NTK
                wup = sbt(es, "wup", [128, 8, 2 * DFF], BF16)
                wdn = sbt(es, "wdn", [128, 22, D], BF16)
                B_wup = [bufs(8, "wup%d_" % hf) for hf in range(2)]
                B_wdn = bufs(4, "wdn")
                xt2 = [sbt(es, "xt%d" % i, [128, 8, NTK], F32) for i in range(2)]; B_xt2 = [bufs(8, "xt%d_" % i) for i in range(2)]
                sq = sbt(es, "sq", [128, 8, NTK], BF16); B_sq = Buf("sq")
                rstd = sbt(es, "rstd", [128, NTK], F32); B_rstd = Buf("rstd")
                hh2 = [sbt(es, "hh%d" % i, [128, 8, NTK], BF16) for i in range(2)]; B_hh2 = [bufs(8, "hh%d_" % i) for i in range(2)]
                NB = 3
                ab = [sbt(es, "ab%d" % i, [128, 2, NTK + 2], F32) for i in range(NB)]; B_ab = bufs(NB, "ab")
                cc = [sbt(es, "cc%d" % i, [128, 2, NTK], F32) for i in range(NB)]; B_cc = [bufs(2, "cc%d_" % i) for i in range(NB)]
                sl = [sbt(es, "sl%d" % i, [128, NTK], F32) for i in range(NB)]; B_sl = bufs(NB, "sl")
                hst = sbt(es, "hst", [128, 22, 2, 2], F32); B_hst = bufs(22, "hst")
                hact = sbt(es, "hact", [128, 22, NTK], BF16); B_hact = bufs(22, "hact")
                rot = Rot([0, 1, 2, 3, 4, 5, 6, 7])

                up_src = I["ffn_w_up"][l].rearrange("(c p) n -> p c n", p=128)
                for grp4 in range(4):
                    for hf in range(2):
                        a = hf * DFF + grp4 * 688
                        DMA("pool", wup[:, :, a:a + 688], up_src[:, :, a:a + 688], (), [B_wup[hf][2 * grp4], B_wup[hf][2 * grp4 + 1]])
                dn_src = I["ffn_w_down"][l]
                for gi, jg in enumerate(range(0, 21, 7)):
                    DMA("pool", wdn[:, jg:jg + 7, :], dn_src[jg * 128:(jg + 7) * 128].rearrange("(j p) n -> p j n", p=128), (), [B_wdn[gi]])
                DMA("pool", wdn[0:64, 21, :], dn_src[2688:2752], (), [B_wdn[3]])
                MEMSET(hst[:].rearrange("p a b c -> p (a b c)"), 0.0, B_hst)
                fcl = lambda idx, k: fcw[:, (l * 44 + idx) * 4 + k:(l * 44 + idx) * 4 + k + 1]
                last = (l == n_layers - 1)

                def wup_bufs(hf, j, rows):
                    g0 = (j * 128) // 344
                    g1 = (j * 128 + rows - 1) // 344
                    return [B_wup[hf][g] for g in range(g0, g1 + 1)]

                def load_x(tt):
                    xi = tt % 2
                    DMA("sp", xt2[xi][:], xs.rearrange("c p t -> p c t")[:, :, tt * NTK:(tt + 1) * NTK], [B_xs[tt * NTK // 512]], B_xt2[xi])

                def norm_sq(tt):
                    ACT(sq[:], xt2[tt % 2][:], AF.Square, B_xt2[tt % 2], [B_sq])

                def norm_pe(tt, kind_l, kind):
                    bk, Bb = rot.next()
                    for c in range(8):
                        MM(bk[:, 0:NTK], ones_bf[:], sq[:, c, :], c == 0, c == 7, [B_sq, B_const], [Bb])
                    ACT(rstd[:], bk[:, 0:NTK], AF.Ln, [Bb], [B_rstd], scale=1.0 / D, bias=EPS)
                    ACT(rstd[:], rstd[:], AF.Exp, [B_rstd], [B_rstd], scale=-0.5)

                def norm_h(tt, c):
                    xi = tt % 2
                    STT(hh2[xi][:, c, :], xt2[xi][:, c, :], gcol(l, 3, c), rstd[:], ALU.mult, ALU.mult,
                        [B_xt2[xi][c], B_rstd, B_const], [B_hh2[xi][c]])

                load_x(0)
                norm_sq(0)
                norm_pe(0, l, 3)
                for c in range(8):
                    norm_h(0, c)
                for tt in range(NT6):
                    t0 = tt * NTK
                    xi = tt % 2
                    xt = xt2[xi]; Bx = B_xt2[xi]
                    hh = hh2[xi]; B_hh = B_hh2[xi]
                    pas = {}

                    def stageA(j):
                        rows = 128 if j < 21 else 64
                        i3 = j % NB
                        pa, Bpa = rot.next()
                        pas[j] = (pa, Bpa)
                        for half in range(2):
                            c0 = half * DFF + j * 128
                            wb = wup_bufs(half, j, rows)
                            for c in range(8):
                                MM(pa[0:rows, half * NTK:(half + 1) * NTK], wup[:, c, c0:c0 + rows], hh[:, c, :], c == 0, c == 7,
                                   wb + [B_hh[c]], [Bpa])
                        abt = ab[i3]
                        ACT(abt[0:rows, :, 2:NTK + 2], pa[0:rows, :].rearrange("p (a b) -> p a b", b=NTK), AF.Copy, [Bpa], [B_ab[i3]])
                        CP(abt[0:rows, :, 0:2], hst[0:rows, j, :, :], [B_hst[j]], [B_ab[i3]])

                    def stageB(j):
                        rows = 128 if j < 21 else 64
                        i3 = j % NB
                        abt = ab[i3]; cct = cc[i3]
                        for half in range(2):
                            idx = half * 22 + j
                            ACT(cct[0:rows, half, :], abt[0:rows, half, 2:NTK + 2], AF.Identity, [B_ab[i3], B_const], [B_cc[i3][half]],
                                scale=fcl(idx, 2)[0:rows], bias=fcl(idx, 3)[0:rows])
                        for k in (1, 0):
                            for half in range(2):
                                idx = half * 22 + j
                                STT(cct[0:rows, half, :], abt[0:rows, half, k:NTK + k], fcl(idx, k)[0:rows], cct[0:rows, half, :], ALU.mult, ALU.add,
                                    [B_ab[i3], B_cc[i3][half], B_const], [B_cc[i3][half]])
                        CP(hst[0:rows, j, :, :], abt[0:rows, :, NTK:NTK + 2], [B_ab[i3]], [B_hst[j]])

                    def stageC(j):
                        rows = 128 if j < 21 else 64
                        i3 = j % NB
                        cct = cc[i3]
                        ACT(sl[i3][0:rows, :], cct[0:rows, 0, :], AF.Silu, [B_cc[i3][0]], [B_sl[i3]])
                        TT(hact[0:rows, j, :], sl[i3][0:rows, :], cct[0:rows, 1, :], ALU.mult, [B_sl[i3], B_cc[i3][1]], [B_hact[j]])

                    for step in range(22 + 2):
                        if step < 22:
                            stageA(step)
                        if 0 <= step - 1 < 22:
                            stageB(step - 1)
                        if 0 <= step - 2 < 22:
                            stageC(step - 2)
                        if tt + 1 < NT6:
                            if step == 4:
                                load_x(tt + 1)
                            if step == 8:
                                norm_sq(tt + 1)
                            if step == 11:
                                norm_pe(tt + 1, l, 3)
                            if 13 <= step < 21:
                                norm_h(tt + 1, step - 13)
                    for oc in range(8):
                        cs = slice(oc * 128, (oc + 1) * 128)
                        pd, Bpd = rot.next()
                        for j in range(22):
                            rows = 128 if j < 21 else 64
                            MM(pd[:, 0:NTK], wdn[0:rows, j, cs], hact[0:rows, j, :], j == 0, j == 21, [B_wdn[min(j // 7, 3)], B_hact[j]], [Bpd])
                        TT(xt[:, oc, :], pd[:, 0:NTK], xt[:, oc, :], ALU.add, [Bpd, Bx[oc]], [Bx[oc]])
                    if not last:
                        DMA("pool", xs.rearrange("c p t -> p c t")[:, :, t0:t0 + NTK], xt[:], Bx, [B_xs[t0 // 512]])
                    else:
                        ACT(hact[:, 0:8, :], xt[:], AF.Square, Bx, B_hact[0:8])
                        bk, Bb = rot.next()
                        for c in range(8):
                            MM(bk[:, 0:NTK], ones_bf[:], hact[:, c, :], c == 0, c == 7, B_hact[0:8] + [B_const], [Bb])
                        ACT(sl[0][:], bk[:, 0:NTK], AF.Ln, [Bb], [B_sl[0]], scale=1.0 / D, bias=EPS)
                        ACT(sl[0][:], sl[0][:], AF.Exp, [B_sl[0]], [B_sl[0]], scale=-0.5)
                        for c in range(8):
                            STT(xt[:, c, :], xt[:, c, :], gcol(L_ALL, 0, c), sl[0][:], ALU.mult, ALU.mult, [Bx[c], B_sl[0], B_const], [Bx[c]])
                        DMA("pool", outT.rearrange("(c p) t -> p c t", p=128)[:, :, t0:t0 + NTK], xt[:], Bx, [Buf()])
                P.flush()
        build.ninstr = P.ninstr
    return nc


_CACHE = {}


def make_in_maps(inputs):
    consts = host_consts()
    params = host_params(inputs)
    shared = {}
    for nm in ["w_in", "w_out", "xattn_wq", "xattn_wkv", "xattn_wo", "ffn_w_up", "ffn_w_down"]:
        shared[nm] = np.ascontiguousarray(inputs[nm], dtype=np.float32)
    shared.update(params)
    shared.update(consts)
    in_maps = []
    for core in range(8):
        b = core % 4
        m = dict(shared)
        m["xT"] = np.ascontiguousarray(inputs["x"][b].T)
        m["memT"] = np.ascontiguousarray(inputs["mem"][b].T)
        in_maps.append(m)
    return in_maps


def kernel(**inputs):
    inputs = {k: np.asarray(v) for k, v in inputs.items()}
    if "nc" not in _CACHE:
        _CACHE["nc"] = build()
    nc = _CACHE["nc"]
    in_maps = make_in_maps(inputs)
    res = run_bass_kernel_spmd(nc, in_maps, core_ids=list(range(8)))
    out = np.stack([np.ascontiguousarray(res.results[b]["outT"].T) for b in range(4)], axis=0)
    return out.astype(np.float32)
```
